# Optimizing a Trainium2 kernel written in Bass

```python
import math
import jax, jax.numpy as jnp
from jax import lax
import numpy as np


D_MODEL = 1024
BATCH = 8
SEQ = 8192
DEPTH = 1

SSM_GROUP = 16
D_SSM = D_MODEL // 2
SSM_GROUPS = D_SSM // SSM_GROUP
SSM_STATE = 64
STEP_MIN = 1e-3
STEP_MAX = 1e-1
HEAD_DIM = 64
D_ATTN = D_MODEL // 2
N_HEADS = D_ATTN // HEAD_DIM
MOBA_BLOCK = 256
MOBA_TOP_K = 3
Q_CHUNK = 32
NUM_BUCKETS = 32
MAX_DISTANCE = 128
N_GROUPS = 4
EXPERTS_PER_GROUP = 8
N_EXPERTS = N_GROUPS * EXPERTS_PER_GROUP
EXPERT_TOP_K = 2
D_EXPERT = D_MODEL // 2
RMS_EPS = 1e-6
NEG_INF = -1e30
D_IN = D_SSM + 3 * D_ATTN + 2 * D_MODEL

kernel_name = 'hybrid_s5_moba_hmoe_block'


def rms_norm(x, g):
    xf = x.astype(jnp.float32)
    y = xf * lax.rsqrt(jnp.mean(xf * xf, axis=-1, keepdims=True) + RMS_EPS)
    return (y * g.astype(jnp.float32)).astype(x.dtype)


def t5_bucket(dist):
    n = jnp.maximum(dist, 0)
    max_exact = NUM_BUCKETS // 2
    large = max_exact + (jnp.log(jnp.maximum(n, max_exact).astype(jnp.float32) / max_exact)
                         / math.log(MAX_DISTANCE / max_exact)
                         * (NUM_BUCKETS - max_exact)).astype(jnp.int32)
    return jnp.where(n < max_exact, n, jnp.minimum(large, NUM_BUCKETS - 1))


def _complex_affine_combine(e1, e2):
    a1r, a1i, b1r, b1i = e1
    a2r, a2i, b2r, b2i = e2
    return (a2r * a1r - a2i * a1i,
            a2r * a1i + a2i * a1r,
            a2r * b1r - a2i * b1i + b2r,
            a2r * b1i + a2i * b1r + b2i)


def s5_mixer(u, lam_re, lam_im, log_step, b_re, b_im, c_re, c_im, d):
    step = jnp.exp(log_step)[:, None]
    decay = jnp.exp(lam_re * step)
    a_re = decay * jnp.cos(lam_im * step)
    a_im = decay * jnp.sin(lam_im * step)
    denom = lam_re * lam_re + lam_im * lam_im
    nr, ni = a_re - 1.0, a_im
    coef_re = (nr * lam_re + ni * lam_im) / denom
    coef_im = (ni * lam_re - nr * lam_im) / denom
    bb_re = coef_re[..., None] * b_re - coef_im[..., None] * b_im
    bb_im = coef_re[..., None] * b_im + coef_im[..., None] * b_re
    bu_re = jnp.einsum('bsgh,gph->bsgp', u, bb_re)
    bu_im = jnp.einsum('bsgh,gph->bsgp', u, bb_im)
    a_re_full = jnp.broadcast_to(a_re, bu_re.shape)
    a_im_full = jnp.broadcast_to(a_im, bu_re.shape)
    _, _, h_re, h_im = lax.associative_scan(
        _complex_affine_combine, (a_re_full, a_im_full, bu_re, bu_im), axis=1)
    y = (jnp.einsum('bsgp,ghp->bsgh', h_re, c_re)
         - jnp.einsum('bsgp,ghp->bsgh', h_im, c_im)
         + d * u)
    return y


def moba_attention(q, k, v, rel_bias):
    B, H, S, Dh = q.shape
    nb = -(-S // MOBA_BLOCK)
    s_pad = nb * MOBA_BLOCK
    pad = ((0, 0), (0, 0), (0, s_pad - S), (0, 0))
    q, k, v = jnp.pad(q, pad), jnp.pad(k, pad), jnp.pad(v, pad)
    scale = HEAD_DIM ** -0.5
    rel_bias = rel_bias.astype(jnp.float32)
    k_blocks = k.reshape(B, H, nb, MOBA_BLOCK, Dh)
    v_blocks = v.reshape(B, H, nb, MOBA_BLOCK, Dh)
    k_mean = jnp.mean(k_blocks.astype(jnp.float32), axis=3)
    gate = jnp.einsum('bhsd,bhnd->bhsn', q.astype(jnp.float32), k_mean)
    q_blk = jnp.arange(s_pad) // MOBA_BLOCK
    gate = jnp.where(jnp.arange(nb)[None, :] < q_blk[:, None], gate, NEG_INF)
    k_top = min(MOBA_TOP_K, nb)
    _, idx = lax.top_k(gate, k_top)
    b_ix = jnp.arange(B)[:, None, None, None]
    h_ix = jnp.arange(H)[None, :, None, None]
    n_sel = k_top * MOBA_BLOCK

    def attend_chunk(c):
        start = c * Q_CHUNK
        own = start // MOBA_BLOCK
        q_c = lax.dynamic_slice_in_dim(q, start, Q_CHUNK, axis=2)
        idx_c = lax.dynamic_slice_in_dim(idx, start, Q_CHUNK, axis=2)
        q_pos = start + jnp.arange(Q_CHUNK)
        k_sel = k_blocks[b_ix, h_ix, idx_c].reshape(B, H, Q_CHUNK, n_sel, Dh)
        v_sel = v_blocks[b_ix, h_ix, idx_c].reshape(B, H, Q_CHUNK, n_sel, Dh)
        sel_pos = (idx_c[..., None] * MOBA_BLOCK + jnp.arange(MOBA_BLOCK)).reshape(B, H, Q_CHUNK, n_sel)
        sel_bias = rel_bias[h_ix, t5_bucket(q_pos[:, None] - sel_pos)]
        valid = jnp.repeat(idx_c < own, MOBA_BLOCK, axis=-1)
        logit_sel = jnp.einsum('bhqd,bhqnd->bhqn', q_c, k_sel).astype(jnp.float32) * scale + sel_bias
        logit_sel = jnp.where(valid, logit_sel, NEG_INF)
        k_own = lax.dynamic_slice_in_dim(k, own * MOBA_BLOCK, MOBA_BLOCK, axis=2)
        v_own = lax.dynamic_slice_in_dim(v, own * MOBA_BLOCK, MOBA_BLOCK, axis=2)
        d_own = q_pos[:, None] - (own * MOBA_BLOCK + jnp.arange(MOBA_BLOCK))[None, :]
        own_bias = rel_bias[:, t5_bucket(d_own)]
        logit_own = jnp.einsum('bhqd,bhkd->bhqk', q_c, k_own).astype(jnp.float32) * scale + own_bias[None]
        logit_own = jnp.where((d_own >= 0)[None, None], logit_own, NEG_INF)
        probs = jax.nn.softmax(jnp.concatenate([logit_sel, logit_own], axis=-1), axis=-1).astype(v.dtype)
        return (jnp.einsum('bhqn,bhqnd->bhqd', probs[..., :n_sel], v_sel)
                + jnp.einsum('bhqk,bhkd->bhqd', probs[..., n_sel:], v_own))

    out = lax.map(attend_chunk, jnp.arange(s_pad // Q_CHUNK))
    out = jnp.moveaxis(out, 0, 2).reshape(B, H, s_pad, Dh)
    return out[:, :, :S]


def hier_moe(h, w_rg, b_rg, w_re, b_re, w1, w3, w2):
    B, S, D = h.shape
    xf = h.reshape(B * S, D)
    g_prob = jax.nn.softmax((xf @ w_rg).astype(jnp.float32) + b_rg.astype(jnp.float32), axis=-1)
    g_val, g_idx = lax.top_k(g_prob, 1)
    e_logits = ((xf @ w_re).astype(jnp.float32) + b_re.astype(jnp.float32)).reshape(-1, N_GROUPS, EXPERTS_PER_GROUP)
    e_sel = jnp.take_along_axis(e_logits, g_idx[:, :, None], axis=1)[:, 0]
    e_val, e_idx = lax.top_k(jax.nn.softmax(e_sel, axis=-1), EXPERT_TOP_K)
    e_val = e_val / jnp.sum(e_val, axis=-1, keepdims=True)
    w_in_group = jnp.einsum('tk,tke->te', e_val, jax.nn.one_hot(e_idx, EXPERTS_PER_GROUP, dtype=jnp.float32))
    combine = (jax.nn.one_hot(g_idx[:, 0], N_GROUPS, dtype=jnp.float32) * g_val)[:, :, None] * w_in_group[:, None, :]
    combine = combine.reshape(-1, N_EXPERTS).astype(xf.dtype)
    y = jnp.zeros_like(xf)
    for e in range(N_EXPERTS):
        hid = jax.nn.silu(xf @ w1[e]) * (xf @ w3[e])
        y = y + combine[:, e:e + 1] * (hid @ w2[e])
    return y.reshape(B, S, D)


def setup_inputs(seed: int = 0) -> dict:
    key = jax.random.key(seed)
    ks = jax.random.split(key, 32)
    f32 = jnp.float32

    def nrm(k, shape, scale):
        return jax.random.normal(k, shape, f32) * scale

    L, G, P, Hc = DEPTH, SSM_GROUPS, SSM_STATE, SSM_GROUP
    return {
        'x': nrm(ks[0], (BATCH, SEQ, D_MODEL), 1.0),
        'ln1_g': 1.0 + nrm(ks[1], (L, D_MODEL), 0.02),
        'w_in': nrm(ks[2], (L, D_MODEL, D_IN), D_MODEL ** -0.5),
        'b_gate': nrm(ks[3], (L, 2 * D_MODEL), 0.1),
        'ssm_lambda_re': -0.5 + nrm(ks[4], (L, G, P), 0.01),
        'ssm_lambda_im': math.pi * jnp.arange(P, dtype=f32) + nrm(ks[5], (L, G, P), 0.01),
        'ssm_log_step': jax.random.uniform(ks[6], (L, G), f32, math.log(STEP_MIN), math.log(STEP_MAX)),
        'ssm_b_re': nrm(ks[7], (L, G, P, Hc), (2 * Hc) ** -0.5),
        'ssm_b_im': nrm(ks[8], (L, G, P, Hc), (2 * Hc) ** -0.5),
        'ssm_c_re': nrm(ks[9], (L, G, Hc, P), P ** -0.5),
        'ssm_c_im': nrm(ks[10], (L, G, Hc, P), P ** -0.5),
        'ssm_d': nrm(ks[11], (L, G, Hc), 1.0),
        'w_glu': nrm(ks[12], (L, D_SSM, D_SSM), D_SSM ** -0.5),
        'b_glu': nrm(ks[13], (L, D_SSM), 0.02),
        'w_up_ssm': nrm(ks[14], (L, D_SSM, D_MODEL), D_SSM ** -0.5),
        'w_up_attn': nrm(ks[15], (L, D_ATTN, D_MODEL), D_ATTN ** -0.5),
        'rel_bias': nrm(ks[16], (N_HEADS, NUM_BUCKETS), 0.5),
        'w_out': nrm(ks[17], (L, D_MODEL, D_MODEL), D_MODEL ** -0.5),
        'ln2_g': 1.0 + nrm(ks[18], (L, D_MODEL), 0.02),
        'w_router_group': nrm(ks[19], (L, D_MODEL, N_GROUPS), D_MODEL ** -0.5),
        'b_router_group': nrm(ks[20], (L, N_GROUPS), 0.01),
        'w_router_expert': nrm(ks[21], (L, D_MODEL, N_EXPERTS), D_MODEL ** -0.5),
        'b_router_expert': nrm(ks[22], (L, N_EXPERTS), 0.01),
        'w1': nrm(ks[23], (L, N_EXPERTS, D_MODEL, D_EXPERT), D_MODEL ** -0.5),
        'w3': nrm(ks[24], (L, N_EXPERTS, D_MODEL, D_EXPERT), D_MODEL ** -0.5),
        'w2': nrm(ks[25], (L, N_EXPERTS, D_EXPERT, D_MODEL), D_EXPERT ** -0.5),
        'ln_f_g': 1.0 + nrm(ks[26], (D_MODEL,), 0.02),
    }


def reference(x, ln1_g, w_in, b_gate, ssm_lambda_re, ssm_lambda_im, ssm_log_step,
              ssm_b_re, ssm_b_im, ssm_c_re, ssm_c_im, ssm_d, w_glu, b_glu,
              w_up_ssm, w_up_attn, rel_bias, w_out, ln2_g, w_router_group,
              b_router_group, w_router_expert, b_router_expert, w1, w3, w2, ln_f_g):
    B, S, D = x.shape
    for l in range(DEPTH):
        h = rms_norm(x, ln1_g[l])
        proj = h @ w_in[l]
        o1 = D_SSM
        o2 = o1 + D_ATTN
        o3 = o2 + D_ATTN
        o4 = o3 + D_ATTN
        u = proj[..., :o1].reshape(B, S, SSM_GROUPS, SSM_GROUP)
        q = proj[..., o1:o2].reshape(B, S, N_HEADS, HEAD_DIM).transpose(0, 2, 1, 3)
        k = proj[..., o2:o3].reshape(B, S, N_HEADS, HEAD_DIM).transpose(0, 2, 1, 3)
        v = proj[..., o3:o4].reshape(B, S, N_HEADS, HEAD_DIM).transpose(0, 2, 1, 3)
        gates = proj[..., o4:] + b_gate[l]
        gate_ssm, gate_attn = gates[..., :D_MODEL], gates[..., D_MODEL:]
        y_ssm = s5_mixer(u, ssm_lambda_re[l], ssm_lambda_im[l], ssm_log_step[l],
                         ssm_b_re[l], ssm_b_im[l], ssm_c_re[l], ssm_c_im[l], ssm_d[l]).reshape(B, S, D_SSM)
        g = jax.nn.gelu(y_ssm)
        y_ssm = g * jax.nn.sigmoid(g @ w_glu[l] + b_glu[l])
        y_attn = moba_attention(q, k, v, rel_bias).transpose(0, 2, 1, 3).reshape(B, S, D_ATTN)
        merged = (jax.nn.sigmoid(gate_ssm) * (y_ssm @ w_up_ssm[l])
                  + jax.nn.sigmoid(gate_attn) * (y_attn @ w_up_attn[l]))
        x = x + merged @ w_out[l]
        x = x + hier_moe(rms_norm(x, ln2_g[l]), w_router_group[l], b_router_group[l],
                         w_router_expert[l], b_router_expert[l], w1[l], w3[l], w2[l])
    return rms_norm(x, ln_f_g)
```

```python
import os
import math
import numpy as np
import ml_dtypes
from contextlib import ExitStack
import concourse.bass as bass
import concourse.mybir as mybir
from concourse.bass_utils import run_bass_kernel_spmd

F32 = mybir.dt.float32
BF16 = mybir.dt.bfloat16
I32 = mybir.dt.int32
AF = mybir.ActivationFunctionType
ALU = mybir.AluOpType
AX = mybir.AxisListType

S = 8192
D = 1024
NT = S // 512
EPS = 1e-6
NEG = -30000.0
COMPUTE = ("pe", "act", "dve", "pool")


class Prog:
    def __init__(self, nc, es):
        self.nc = nc
        self.es = es
        self.ops = {e: [] for e in COMPUTE + ("sp",)}
        self.psem = {e: es.enter_context(nc.semaphore("prog_" + e)) for e in COMPUTE}
        self.cnt = {e: 0 for e in COMPUTE}
        self.dsem = {}
        self.dcnt = {}
        self.res = {}
        self.waited = {e: {} for e in self.ops}
        self.free = {True: [], False: []}
        self.dkind = {}

    def _deps(self, eng, reads, writes):
        need = {}

        def add(tok):
            if tok is None:
                return
            sem, val, src = tok
            if src == "pe" and eng == "pe":
                return
            k = id(sem)
            if k not in need or need[k][1] < val:
                need[k] = (sem, val)

        for r in reads:
            st = self.res.get(r)
            if st:
                add(st[0])
        for r in writes:
            st = self.res.get(r)
            if st:
                add(st[0])
                for t in st[1]:
                    add(t)
        out = []
        for k, (sem, val) in need.items():
            if self.waited[eng].get(k, 0) >= val:
                continue
            self.waited[eng][k] = val
            out.append((sem, val))
        return out

    def _commit(self, tok, reads, writes):
        for r in writes:
            self.res[r] = [tok, []]
        for r in reads:
            if r in writes:
                continue
            st = self.res.setdefault(r, [None, []])
            st[1].append(tok)
            if len(st[1]) > 64:
                st[1] = st[1][-64:]

    def op(self, eng, fn, reads=(), writes=()):
        waits = self._deps(eng, reads, writes)
        self.cnt[eng] += 1
        tok = (self.psem[eng], self.cnt[eng], eng)
        self.ops[eng].append((waits, fn, (self.psem[eng], 1)))
        self._commit(tok, reads, writes)

    def dma(self, eng, fn, key, reads=(), writes=()):
        if key not in self.dsem:
            kind = (eng == "pool")
            self.dkind[key] = kind
            if self.free[kind]:
                self.dsem[key], self.dcnt[key] = self.free[kind].pop()
            else:
                self.dsem[key] = self.es.enter_context(self.nc.semaphore("d_" + key))
                self.dcnt[key] = 0
        assert self.dkind[key] == (eng == "pool"), key
        waits = self._deps(eng, reads, writes)
        self.dcnt[key] += 16
        tok = (self.dsem[key], self.dcnt[key], "dma")
        self.ops[eng].append((waits, fn, (self.dsem[key], 16)))
        self._commit(tok, reads, writes)

    def barrier(self):
        for e in self.ops:
            waits = []
            for c in COMPUTE:
                if self.cnt[c] > 0 and self.waited[e].get(id(self.psem[c]), 0) < self.cnt[c]:
                    self.waited[e][id(self.psem[c])] = self.cnt[c]
                    waits.append((self.psem[c], self.cnt[c]))
            for k, sem in self.dsem.items():
                if self.waited[e].get(id(sem), 0) < self.dcnt[k]:
                    self.waited[e][id(sem)] = self.dcnt[k]
                    waits.append((sem, self.dcnt[k]))
            if waits:
                self.ops[e].append((waits, None, None))
        self.res = {}
        for k in list(self.dsem):
            self.free[self.dkind.pop(k)].append((self.dsem.pop(k), self.dcnt.pop(k)))

    def emit(self):
        nc = self.nc
        ops = self.ops

        def mk(name):
            def f(eng):
                for waits, fn, inc in ops[name]:
                    for sem, val in waits:
                        eng.wait_ge(sem, val)
                    if fn is not None:
                        ins = fn(eng)
                        ins.then_inc(inc[0], inc[1])
            return f

        with nc.Block() as blk:
            blk.sync(mk("sp"))
            blk.scalar(mk("act"))
            blk.vector(mk("dve"))
            blk.gpsimd(mk("pool"))
            blk.tensor(mk("pe"))
        self.ops = {e: [] for e in self.ops}


def build(dbg=None):
    nc = bass.Bass("TRN2", target_bir_lowering=False)
    es = ExitStack()
    P = Prog(nc, es)

    def din(name, shape, dt=F32):
        return nc.dram_tensor(name, list(shape), dt, kind="ExternalInput").ap()

    def dscr(name, shape, dt):
        kind = "ExternalOutput" if (dbg and name in dbg) else "Internal"
        return nc.dram_tensor(name, list(shape), dt, kind=kind).ap()

    def sb(name, shape, dt, st=None):
        return (st or es).enter_context(nc.sbuf_tensor(name, list(shape), dt))

    x_d = din("x", [S, D])
    win_d = din("w_in", [D, 4096])
    g1_d = din("g1", [128, 8])
    bg_d = din("bgate", [128, 16])
    identb_d = din("identb", [128, 128], BF16)
    out_d = nc.dram_tensor("out", [S, D], F32, kind="ExternalOutput").ap()

    uT_d = dscr("uT", [4, 128, S], BF16)
    qT_d = dscr("qT", [4, 128, S], BF16)
    kT_d = dscr("kT", [4, 128, S], BF16)
    v_d = dscr("v", [S, 512], BF16)
    gate_d = dscr("gate", [16, 128, S], BF16)

    ps = [es.enter_context(nc.psum_tensor("ps%d" % i, [128, 512], F32)) for i in range(8)]
    identb = sb("identb_s", [128, 128], BF16)
    P.dma("sp", lambda e: e.dma_start(out=identb[:], in_=identb_d), "ident", writes=["identb"])
    identr_d = din("identr", [128, 128], BF16)
    identr = sb("identr_s", [128, 128], BF16)
    P.dma("sp", lambda e: e.dma_start(out=identr[:], in_=identr_d), "identr", writes=["identr"])

    with ExitStack() as st:
        winb = sb("winb", [128, 8, 4096], BF16, st)
        wstage = [sb("wstage%d" % i, [128, 4096], F32, st) for i in range(2)]
        g1 = sb("g1s", [128, 8], F32, st)
        bg = sb("bgs", [128, 16], F32, st)
        xs = [sb("xs%d" % i, [128, 4, 1024], F32, st) for i in range(2)]
        junk = sb("junk", [128, 1024], BF16, st)
        ss = sb("ss", [128, 4], F32, st)
        rstd = sb("rstd", [128, 4], F32, st)
        xnb = sb("xnb", [128, 4, 1024], BF16, st)
        hT = sb("hT", [128, 8, 512], BF16, st)
        NST = 8
        stg = [sb("stg%d" % i, [128, 512], BF16, st) for i in range(NST)]

        P.dma("sp", lambda e: e.dma_start(out=g1[:], in_=g1_d), "g1", writes=["g1"])
        P.dma("sp", lambda e: e.dma_start(out=bg[:], in_=bg_d), "bg", writes=["bg"])
        for kc in range(8):
            w = wstage[kc % 2]
            P.dma("sp", lambda e, w=w, kc=kc: e.dma_start(out=w[:], in_=win_d[kc * 128:(kc + 1) * 128, :]),
                  "wst%d" % (kc % 2), writes=["wstage%d" % (kc % 2)])
            P.op("dve", lambda e, w=w, kc=kc: e.tensor_scalar(out=winb[:, kc, :], in0=w[:], scalar1=g1[:, kc:kc + 1],
                                                             scalar2=None, op0=ALU.mult),
                 reads=["wstage%d" % (kc % 2), "g1"], writes=["winb"])

        x_t = x_d.rearrange("(t j p) d -> t p j d", j=4, p=128)

        def load_x(T):
            P.dma("sp", lambda e, T=T: e.dma_start(out=xs[T % 2][:], in_=x_t[T]), "xs%d" % (T % 2),
                  writes=["xs%d" % (T % 2)])

        load_x(0)
        nst = 0
        evac = 0
        for T in range(NT):
            if T + 1 < NT:
                load_x(T + 1)
            xt = xs[T % 2]
            xr = "xs%d" % (T % 2)
            for j in range(4):
                P.op("act", lambda e, xt=xt, j=j: e.activation(out=junk[:], in_=xt[:, j, :], func=AF.Square,
                                                               accum_out=ss[:, j:j + 1]),
                     reads=[xr], writes=["junk", "ss"])
            P.op("act", lambda e: e.activation(out=rstd[:], in_=ss[:], func=AF.Sqrt, scale=1.0 / D, bias=EPS),
                 reads=["ss"], writes=["rstd"])
            P.op("dve", lambda e: e.reciprocal(out=rstd[:], in_=rstd[:]), reads=["rstd"], writes=["rstd"])
            for j in range(4):
                P.op("dve", lambda e, xt=xt, j=j: e.tensor_scalar(out=xnb[:, j, :], in0=xt[:, j, :],
                                                                 scalar1=rstd[:, j:j + 1], scalar2=None, op0=ALU.mult),
                     reads=[xr, "rstd"], writes=["xnb%d" % j])
            for kc in range(8):
                bank = ps[kc % 2]
                pv = bank[:].bitcast(BF16)
                for j in range(4):
                    P.op("pe", lambda e, pv=pv, j=j, kc=kc: e.transpose(out=pv[:, j * 128:(j + 1) * 128],
                                                                        in_=xnb[:, j, kc * 128:(kc + 1) * 128],
                                                                        identity=identb[:]),
                         reads=["xnb%d" % j, "identb"], writes=["psA%d" % (kc % 2)])
                eng = "act" if kc % 2 == 0 else "dve"
                if eng == "act":
                    P.op("act", lambda e, pv=pv, kc=kc: e.copy(out=hT[:, kc, :], in_=pv[:, 0:512]),
                         reads=["psA%d" % (kc % 2)], writes=["hT%d" % kc])
                else:
                    P.op("dve", lambda e, pv=pv, kc=kc: e.tensor_copy(out=hT[:, kc, :], in_=pv[:, 0:512]),
                         reads=["psA%d" % (kc % 2)], writes=["hT%d" % kc])
            hres = ["hT%d" % kc for kc in range(8)]
            for fc in list(range(0, 12)) + list(range(16, 32)):
                bank = ps[2 + (evac % 4)]
                br = "psB%d" % (evac % 4)
                for kc in range(8):
                    P.op("pe", lambda e, bank=bank, kc=kc, fc=fc: e.matmul(bank[:], lhsT=winb[:, kc, fc * 128:(fc + 1) * 128],
                                                                            rhs=hT[:, kc, :], start=(kc == 0), stop=(kc == 7)),
                         reads=hres + ["winb"] if kc == 0 else [], writes=[br])
                so = stg[nst % NST]
                sr = "stg%d" % (nst % NST)
                if fc < 4:
                    dst = uT_d[fc, :, T * 512:(T + 1) * 512]
                elif fc < 8:
                    dst = qT_d[fc - 4, :, T * 512:(T + 1) * 512]
                elif fc < 12:
                    dst = kT_d[fc - 8, :, T * 512:(T + 1) * 512]
                else:
                    dst = gate_d[fc - 16, :, T * 512:(T + 1) * 512]
                if fc >= 16:
                    P.op("act", lambda e, bank=bank, so=so, fc=fc: e.activation(out=so[:], in_=bank[:], func=AF.Sigmoid,
                                                                                bias=bg[:, fc - 16:fc - 15], scale=1.0),
                         reads=[br, "bg"], writes=[sr])
                elif 4 <= fc < 8:
                    P.op("dve", lambda e, bank=bank, so=so: e.tensor_scalar(out=so[:], in0=bank[:], scalar1=0.125,
                                                                            scalar2=None, op0=ALU.mult),
                         reads=[br], writes=[sr])
                else:
                    P.op("dve", lambda e, bank=bank, so=so: e.tensor_copy(out=so[:], in_=bank[:]),
                         reads=[br], writes=[sr])
                P.dma("sp", lambda e, so=so, dst=dst: e.dma_start(out=dst, in_=so[:]), sr + "o", reads=[sr])
                nst += 1
                evac += 1
            for j in range(4):
                bank = ps[2 + (evac % 4)]
                br = "psB%d" % (evac % 4)
                for kc in range(8):
                    P.op("pe", lambda e, bank=bank, kc=kc, j=j: e.matmul(bank[:], lhsT=hT[:, kc, j * 128:(j + 1) * 128],
                                                                          rhs=winb[:, kc, 1536:2048], start=(kc == 0), stop=(kc == 7)),
                         reads=hres + ["winb"] if kc == 0 else [], writes=[br])
                so = stg[nst % NST]
                sr = "stg%d" % (nst % NST)
                P.op("act", lambda e, bank=bank, so=so: e.copy(out=so[:], in_=bank[:]), reads=[br], writes=[sr])
                r0 = T * 512 + j * 128
                P.dma("sp", lambda e, so=so, r0=r0: e.dma_start(out=v_d[r0:r0 + 128, :], in_=so[:]), sr + "o", reads=[sr])
                nst += 1
                evac += 1
        P.barrier()
        P.emit()

    if dbg and "stopA" in dbg:
        es.close()
        return nc

    y2T_d = dscr("y2T", [4, 128, S], BF16)
    w1_d = din("w1", [32, 1024, 512]); w3_d = din("w3", [32, 1024, 512]); w2_d = din("w2", [32, 512, 1024])
    Xs_d = dscr("Xs", [64 * 512, D], BF16)
    w1b_d = dscr("w1b", [4096, 4096], BF16); w3b_d = dscr("w3b", [4096, 4096], BF16); w2b_d = dscr("w2b", [4096, 4096], BF16)
    w1v = w1_d.rearrange("e (p kc) n -> (e p) (kc n)", kc=8)
    w3v = w3_d.rearrange("e (p kc) n -> (e p) (kc n)", kc=8)
    w2v = w2_d.rearrange("e (p fc) n -> (e p) (fc n)", fc=4)
    PRM = {}
    for nm, shp in (("lre_ml", [128, 16]), ("lim_ml", [128, 16]), ("lst_ml", [128, 16]),
                    ("lre_fl", [128, 2048]), ("lim_fl", [128, 2048]), ("lst_fl", [128, 2048]),
                    ("bre_fl", [128, 2048]), ("bim_fl", [128, 2048]),
                    ("cre_ml", [128, 2048]), ("cim_ml", [128, 2048]),
                    ("d_fm", [128, 4]), ("wglu", [512, 512]), ("bglu", [128, 4])):
        PRM[nm] = din(nm, shp)
    with ExitStack() as st:
        CH = 256
        NCH = S // CH

        def V(fn, r, w):
            P.op("dve", fn, reads=r, writes=w)

        def A(fn, r, w):
            P.op("act", fn, reads=r, writes=w)

        def G(fn, r, w):
            P.op("pool", fn, reads=r, writes=w)

        def load(nm, shape, src=None, dt=F32, stk=None):
            t = sb("B_" + nm, shape, dt, stk or st)
            P.dma("sp", lambda e: e.dma_start(out=t[:], in_=(src if src is not None else PRM[nm])), "B_" + nm,
                  writes=["B_" + nm])
            return t

        def rot_params(tag, lre, lim, lst, n, stk=None):
            mk = lambda s_: sb("B_%s_%s" % (tag, s_), [128, n], F32, stk or st)
            nm = lambda s_: "B_%s_%s" % (tag, s_)
            step, r, th, c, s, t1, t2 = [mk(x) for x in ("step", "r", "th", "c", "s", "t1", "t2")]
            lren, limn, lstn = ["B_" + x for x in (lre[1], lim[1], lst[1])]
            lre, lim, lst = lre[0], lim[0], lst[0]
            A(lambda e: e.activation(out=step[:], in_=lst[:], func=AF.Exp), [lstn], [nm("step")])
            V(lambda e: e.tensor_tensor(out=th[:], in0=lim[:], in1=step[:], op=ALU.mult), [limn, nm("step")], [nm("th")])
            V(lambda e: e.tensor_tensor(out=t1[:], in0=lre[:], in1=step[:], op=ALU.mult), [lren, nm("step")], [nm("t1")])
            A(lambda e: e.activation(out=r[:], in_=t1[:], func=AF.Exp), [nm("t1")], [nm("r")])
            A(lambda e: e.activation(out=s[:], in_=th[:], func=AF.Sin, scale=1.0 / 16), [nm("th")], [nm("s")])
            V(lambda e: e.tensor_scalar(out=t2[:], in0=th[:], scalar1=1.0 / 16, scalar2=math.pi / 2, op0=ALU.mult, op1=ALU.add),
              [nm("th")], [nm("t2")])
            A(lambda e: e.activation(out=c[:], in_=t2[:], func=AF.Sin), [nm("t2")], [nm("c")])
            for _ in range(4):
                V(lambda e: e.tensor_tensor(out=t1[:], in0=c[:], in1=c[:], op=ALU.mult), [nm("c")], [nm("t1")])
                V(lambda e: e.tensor_tensor(out=t2[:], in0=s[:], in1=s[:], op=ALU.mult), [nm("s")], [nm("t2")])
                V(lambda e: e.scalar_tensor_tensor(out=s[:], in0=s[:], scalar=2.0, in1=c[:], op0=ALU.mult, op1=ALU.mult),
                  [nm("s"), nm("c")], [nm("s")])
                V(lambda e: e.tensor_tensor(out=c[:], in0=t1[:], in1=t2[:], op=ALU.subtract), [nm("t1"), nm("t2")], [nm("c")])
            return dict(r=r, c=c, s=s, t1=t1, t2=t2, rn=nm("r"), cn=nm("c"), sn=nm("s"), t1n=nm("t1"), t2n=nm("t2"), th=th, thn=nm("th"), step=step, stepn=nm("step"))

        lre_ml = load("lre_ml", [128, 16]); lim_ml = load("lim_ml", [128, 16]); lst_ml = load("lst_ml", [128, 16])
        ml = rot_params("ml", (lre_ml, "lre_ml"), (lim_ml, "lim_ml"), (lst_ml, "lst_ml"), 16)
        Ec = sb("B_Ec", [128, 16, CH], F32, st)
        Es = sb("B_Es", [128, 16, CH], F32, st)
        pc = sb("B_pc", [128, 16], F32, st)
        psn = sb("B_psn", [128, 16], F32, st)
        bbre = sb("B_bbre", [128, 16, 128], BF16, st)
        bbim = sb("B_bbim", [128, 16, 128], BF16, st)
        cmre = sb("B_cmre", [128, 16, 128], BF16, st)
        cmim = sb("B_cmim", [128, 16, 128], BF16, st)
        cmren = sb("B_cmren", [128, 16, 128], BF16, st)
        dfm = load("d_fm", [128, 4])
        bglu = load("bglu", [128, 4])
        wglu = sb("B_wglu", [128, 4, 512], BF16, st)
        stp = ExitStack()
        tq1 = sb("B_tq1", [128, 16, CH // 2], F32, stp)
        tq2 = sb("B_tq2", [128, 16, CH // 2], F32, stp)
        V(lambda e: e.memset(Ec[:, :, 0:1], 1.0), [], ["B_Ec"])
        V(lambda e: e.memset(Es[:, :, 0:1], 0.0), [], ["B_Es"])
        V(lambda e: e.tensor_copy(out=pc[:], in_=ml["c"][:]), [ml["cn"]], ["B_pc"])
        V(lambda e: e.tensor_copy(out=psn[:], in_=ml["s"][:]), [ml["sn"]], ["B_psn"])
        n = 1
        while n < CH:
            bcC = pc[:, :].unsqueeze(2).to_broadcast([128, 16, n])
            bcS = psn[:, :].unsqueeze(2).to_broadcast([128, 16, n])
            a1 = tq1[:, :, 0:n]
            a2 = tq2[:, :, 0:n]
            V(lambda e, bcC=bcC, a1=a1, n=n: e.tensor_tensor(out=a1, in0=Ec[:, :, 0:n], in1=bcC, op=ALU.mult), ["B_Ec", "B_pc"], ["B_tq1"])
            V(lambda e, bcS=bcS, a2=a2, n=n: e.tensor_tensor(out=a2, in0=Es[:, :, 0:n], in1=bcS, op=ALU.mult), ["B_Es", "B_psn"], ["B_tq2"])
            V(lambda e, a1=a1, a2=a2, n=n: e.tensor_tensor(out=Ec[:, :, n:2 * n], in0=a1, in1=a2, op=ALU.subtract), ["B_tq1", "B_tq2"], ["B_Ec"])
            V(lambda e, bcS=bcS, a1=a1, n=n: e.tensor_tensor(out=a1, in0=Ec[:, :, 0:n], in1=bcS, op=ALU.mult), ["B_Ec", "B_psn"], ["B_tq1"])
            V(lambda e, bcC=bcC, a2=a2, n=n: e.tensor_tensor(out=a2, in0=Es[:, :, 0:n], in1=bcC, op=ALU.mult), ["B_Es", "B_pc"], ["B_tq2"])
            V(lambda e, a1=a1, a2=a2, n=n: e.tensor_tensor(out=Es[:, :, n:2 * n], in0=a1, in1=a2, op=ALU.add), ["B_tq1", "B_tq2"], ["B_Es"])
            mt1, mt2 = ml["t1"], ml["t2"]
            V(lambda e, mt1=mt1: e.tensor_tensor(out=mt1[:], in0=pc[:], in1=pc[:], op=ALU.mult), ["B_pc"], [ml["t1n"]])
            V(lambda e, mt2=mt2: e.tensor_tensor(out=mt2[:], in0=psn[:], in1=psn[:], op=ALU.mult), ["B_psn"], [ml["t2n"]])
            V(lambda e: e.scalar_tensor_tensor(out=psn[:], in0=psn[:], scalar=2.0, in1=pc[:], op0=ALU.mult, op1=ALU.mult), ["B_psn", "B_pc"], ["B_psn"])
            V(lambda e, mt1=mt1, mt2=mt2: e.tensor_tensor(out=pc[:], in0=mt1[:], in1=mt2[:], op=ALU.subtract), [ml["t1n"], ml["t2n"]], ["B_pc"])
            n *= 2
        lre_fl = load("lre_fl", [128, 2048], stk=stp); lim_fl = load("lim_fl", [128, 2048], stk=stp); lst_fl = load("lst_fl", [128, 2048], stk=stp)
        fl = rot_params("fl", (lre_fl, "lre_fl"), (lim_fl, "lim_fl"), (lst_fl, "lst_fl"), 2048, stk=stp)
        bre = load("bre_fl", [128, 2048], stk=stp); bim = load("bim_fl", [128, 2048], stk=stp)
        nr = fl["step"]; nrn = fl["stepn"]
        den = fl["th"]; denn = fl["thn"]
        t1, t2, c_, s_, r_ = fl["t1"], fl["t2"], fl["c"], fl["s"], fl["r"]
        t1n, t2n, cn, sn, rn = fl["t1n"], fl["t2n"], fl["cn"], fl["sn"], fl["rn"]
        V(lambda e: e.tensor_tensor(out=c_[:], in0=c_[:], in1=r_[:], op=ALU.mult), [cn, rn], [cn])
        V(lambda e: e.tensor_tensor(out=s_[:], in0=s_[:], in1=r_[:], op=ALU.mult), [sn, rn], [sn])
        V(lambda e: e.tensor_scalar(out=nr[:], in0=c_[:], scalar1=-1.0, scalar2=None, op0=ALU.add), [cn], [nrn])
        V(lambda e: e.tensor_tensor(out=t1[:], in0=lre_fl[:], in1=lre_fl[:], op=ALU.mult), ["B_lre_fl"], [t1n])
        V(lambda e: e.tensor_tensor(out=t2[:], in0=lim_fl[:], in1=lim_fl[:], op=ALU.mult), ["B_lim_fl"], [t2n])
        V(lambda e: e.tensor_tensor(out=den[:], in0=t1[:], in1=t2[:], op=ALU.add), [t1n, t2n], [denn])
        V(lambda e: e.reciprocal(out=den[:], in_=den[:]), [denn], [denn])
        V(lambda e: e.tensor_tensor(out=t1[:], in0=nr[:], in1=lre_fl[:], op=ALU.mult), [nrn, "B_lre_fl"], [t1n])
        V(lambda e: e.tensor_tensor(out=t2[:], in0=s_[:], in1=lim_fl[:], op=ALU.mult), [sn, "B_lim_fl"], [t2n])
        V(lambda e: e.tensor_tensor(out=r_[:], in0=t1[:], in1=t2[:], op=ALU.add), [t1n, t2n], [rn])
        V(lambda e: e.tensor_tensor(out=r_[:], in0=r_[:], in1=den[:], op=ALU.mult), [rn, denn], [rn])
        V(lambda e: e.tensor_tensor(out=t1[:], in0=s_[:], in1=lre_fl[:], op=ALU.mult), [sn, "B_lre_fl"], [t1n])
        V(lambda e: e.tensor_tensor(out=t2[:], in0=nr[:], in1=lim_fl[:], op=ALU.mult), [nrn, "B_lim_fl"], [t2n])
        V(lambda e: e.tensor_tensor(out=c_[:], in0=t1[:], in1=t2[:], op=ALU.subtract), [t1n, t2n], [cn])
        V(lambda e: e.tensor_tensor(out=c_[:], in0=c_[:], in1=den[:], op=ALU.mult), [cn, denn], [cn])
        bbre_f = bbre[:].rearrange("p k m -> p (k m)")
        bbim_f = bbim[:].rearrange("p k m -> p (k m)")
        V(lambda e: e.tensor_tensor(out=t1[:], in0=r_[:], in1=bre[:], op=ALU.mult), [rn, "B_bre_fl"], [t1n])
        V(lambda e: e.tensor_tensor(out=t2[:], in0=c_[:], in1=bim[:], op=ALU.mult), [cn, "B_bim_fl"], [t2n])
        V(lambda e: e.tensor_tensor(out=bbre_f, in0=t1[:], in1=t2[:], op=ALU.subtract), [t1n, t2n], ["B_bbre"])
        V(lambda e: e.tensor_tensor(out=t1[:], in0=r_[:], in1=bim[:], op=ALU.mult), [rn, "B_bim_fl"], [t1n])
        V(lambda e: e.tensor_tensor(out=t2[:], in0=c_[:], in1=bre[:], op=ALU.mult), [cn, "B_bre_fl"], [t2n])
        V(lambda e: e.tensor_tensor(out=bbim_f, in0=t1[:], in1=t2[:], op=ALU.add), [t1n, t2n], ["B_bbim"])
        P.dma("sp", lambda e: e.dma_start(out=lre_fl[:], in_=PRM["cre_ml"]), "B_lre_fl", writes=["B_lre_fl"])
        P.dma("sp", lambda e: e.dma_start(out=lim_fl[:], in_=PRM["cim_ml"]), "B_lim_fl", writes=["B_lim_fl"])
        V(lambda e: e.tensor_copy(out=cmre[:].rearrange("p k m -> p (k m)"), in_=lre_fl[:]), ["B_lre_fl"], ["B_cmre"])
        V(lambda e: e.tensor_scalar(out=cmren[:].rearrange("p k m -> p (k m)"), in0=lre_fl[:], scalar1=-1.0, scalar2=None, op0=ALU.mult), ["B_lre_fl"], ["B_cmren"])
        V(lambda e: e.tensor_scalar(out=cmim[:].rearrange("p k m -> p (k m)"), in0=lim_fl[:], scalar1=-1.0, scalar2=None, op0=ALU.mult), ["B_lim_fl"], ["B_cmim"])
        P.dma("sp", lambda e: e.dma_start(out=lst_fl[:].rearrange("p (k n) -> p k n", k=4), in_=PRM["wglu"].rearrange("(k p) n -> p k n", p=128)),
              "B_lst_fl", writes=["B_lst_fl"])
        V(lambda e: e.tensor_copy(out=wglu[:].rearrange("p k n -> p (k n)"), in_=lst_fl[:]), ["B_lst_fl"], ["B_wglu"])

        P.barrier()
        P.emit()
        stp.close()
        ub = [sb("B_u%d" % i, [128, 4, CH], BF16, st) for i in range(2)]
        gre = sb("B_gre", [128, 16, CH], F32, st)
        gim = sb("B_gim", [128, 16, CH], F32, st)
        ini_re = sb("B_inire", [128, 16], F32, st)
        ini_im = sb("B_iniim", [128, 16], F32, st)
        i1 = sb("B_i1", [128, 16], F32, st)
        i2 = sb("B_i2", [128, 16], F32, st)
        V(lambda e: e.memset(ini_re[:], 0.0), [], ["B_inire"])
        V(lambda e: e.memset(ini_im[:], 0.0), [], ["B_iniim"])
        NB = 2
        w1 = [sb("B_w1_%d" % i, [128, CH], F32, st) for i in range(NB)]
        w2 = [sb("B_w2_%d" % i, [128, CH], F32, st) for i in range(NB)]
        gi_re = [sb("B_gire%d" % i, [128, CH], F32, st) for i in range(NB)]
        gi_im = [sb("B_giim%d" % i, [128, CH], F32, st) for i in range(NB)]
        q1 = [sb("B_q1_%d" % i, [128, CH], F32, st) for i in range(NB)]
        q2 = [sb("B_q2_%d" % i, [128, CH], F32, st) for i in range(NB)]
        prd = [[sb("B_prd%d_%d" % (i, c), [128, CH], BF16, st) for c in range(4)] for i in range(4)]
        ysb = [sb("B_y%d" % i, [128, CH], F32, st) for i in range(2)]
        yt = [sb("B_yt%d" % i, [128, CH], F32, st) for i in range(2)]
        ysg = [sb("B_ysg%d" % i, [128, CH], F32, st) for i in range(2)]
        gb = [sb("B_g%d" % i, [128, 4, CH], BF16, st) for i in range(2)]
        zs = [sb("B_z%d" % i, [128, CH], F32, st) for i in range(2)]
        y2 = [sb("B_y2_%d" % i, [128, 4, CH], BF16, st) for i in range(2)]
        uT_v = uT_d.rearrange("k p s -> p k s")
        y2_v = y2T_d.rearrange("k p s -> p k s")

        def load_u(ch):
            P.dma("sp", lambda e, ch=ch: e.dma_start(out=ub[ch % 2][:], in_=uT_v[:, :, ch * CH:(ch + 1) * CH]),
                  "B_u%d" % (ch % 2), writes=["B_u%d" % (ch % 2)])

        SQ = math.sqrt(0.044715)

        def emit_Bu(ch, k):
            u = ub[ch % 2]
            un = "B_u%d" % (ch % 2)
            kq = k // 4
            bank = ps[k % 2]
            bn = "psB%d" % (k % 2)
            P.op("pe", lambda e: e.matmul(bank[:, 0:CH], lhsT=bbre[:, k, :], rhs=u[:, kq, :], start=True, stop=True),
                 reads=[un, "B_bbre"], writes=[bn])
            P.op("pe", lambda e: e.matmul(bank[:, CH:2 * CH], lhsT=bbim[:, k, :], rhs=u[:, kq, :], start=True, stop=True),
                 reads=[un, "B_bbim"], writes=[bn])

        def emit_mid(ch, k, hb):
            b = k % NB
            bank = ps[k % 2]
            bn = "psB%d" % (k % 2)
            Bre = bank[:, 0:CH]
            Bim = bank[:, CH:2 * CH]
            ec = Ec[:, k, :]
            esn = Es[:, k, :]
            n1, n2, ngr, ngi = "B_w1_%d" % b, "B_w2_%d" % b, "B_gire%d" % b, "B_giim%d" % b
            V(lambda e: e.tensor_tensor(out=w1[b][:], in0=Bre, in1=ec, op=ALU.mult), [bn, "B_Ec"], [n1])
            V(lambda e: e.tensor_tensor(out=w2[b][:], in0=Bim, in1=esn, op=ALU.mult), [bn, "B_Es"], [n2])
            V(lambda e: e.tensor_tensor(out=gi_re[b][:], in0=w1[b][:], in1=w2[b][:], op=ALU.add), [n1, n2], [ngr])
            V(lambda e: e.tensor_tensor(out=w1[b][:], in0=Bim, in1=ec, op=ALU.mult), [bn, "B_Ec"], [n1])
            V(lambda e: e.tensor_tensor(out=w2[b][:], in0=Bre, in1=esn, op=ALU.mult), [bn, "B_Es"], [n2])
            V(lambda e: e.tensor_tensor(out=gi_im[b][:], in0=w1[b][:], in1=w2[b][:], op=ALU.subtract), [n1, n2], [ngi])
            rb = ml["r"][:, k:k + 1].to_broadcast([128, CH])
            V(lambda e: e.tensor_tensor_scan(out=gre[:, k, :], data0=rb, data1=gi_re[b][:], initial=ini_re[:, k:k + 1],
                                             op0=ALU.mult, op1=ALU.add), [ngr, ml["rn"], "B_inire"], ["B_gre%d" % k])
            V(lambda e: e.tensor_tensor_scan(out=gim[:, k, :], data0=rb, data1=gi_im[b][:], initial=ini_im[:, k:k + 1],
                                             op0=ALU.mult, op1=ALU.add), [ngi, ml["rn"], "B_iniim"], ["B_gim%d" % k])
            pr = prd[hb]
            G(lambda e: e.tensor_tensor(out=pr[0][:], in0=gre[:, k, :], in1=ec, op=ALU.mult), ["B_gre%d" % k, "B_Ec"], ["B_prd%d_0" % hb])
            G(lambda e: e.tensor_tensor(out=pr[1][:], in0=gim[:, k, :], in1=esn, op=ALU.mult), ["B_gim%d" % k, "B_Es"], ["B_prd%d_1" % hb])
            G(lambda e: e.tensor_tensor(out=pr[2][:], in0=gre[:, k, :], in1=esn, op=ALU.mult), ["B_gre%d" % k, "B_Es"], ["B_prd%d_2" % hb])
            G(lambda e: e.tensor_tensor(out=pr[3][:], in0=gim[:, k, :], in1=ec, op=ALU.mult), ["B_gim%d" % k, "B_Ec"], ["B_prd%d_3" % hb])

        def emit_C(ch, k, hb):
            kq = k // 4
            ybank = ps[2 + kq]
            ycols = slice((ch % 2) * CH, (ch % 2) * CH + CH)
            yn = "psY%d_%d" % (kq, ch % 2)
            first = (k % 4 == 0)
            last = (k % 4 == 3)
            pr = prd[hb]
            P.op("pe", lambda e: e.matmul(ybank[:, ycols], lhsT=cmre[:, k, :], rhs=pr[0][:], start=first, stop=False),
                 reads=["B_prd%d_0" % hb, "B_cmre"], writes=[yn])
            P.op("pe", lambda e: e.matmul(ybank[:, ycols], lhsT=cmren[:, k, :], rhs=pr[1][:], start=False, stop=False),
                 reads=["B_prd%d_1" % hb, "B_cmren"], writes=[yn])
            P.op("pe", lambda e: e.matmul(ybank[:, ycols], lhsT=cmim[:, k, :], rhs=pr[2][:], start=False, stop=False),
                 reads=["B_prd%d_2" % hb, "B_cmim"], writes=[yn])
            P.op("pe", lambda e: e.matmul(ybank[:, ycols], lhsT=cmim[:, k, :], rhs=pr[3][:], start=False, stop=last),
                 reads=["B_prd%d_3" % hb, "B_cmim"], writes=[yn])

        def emit_epi(ch, kq):
            u = ub[ch % 2]
            un = "B_u%d" % (ch % 2)
            ybank = ps[2 + kq]
            ycols = slice((ch % 2) * CH, (ch % 2) * CH + CH)
            yn = "psY%d_%d" % (kq, ch % 2)
            yb = kq % 2
            ynm, ytn, ysn = "B_y%d" % yb, "B_yt%d" % yb, "B_ysg%d" % yb
            gbn = "B_g%d_%d" % (ch % 2, kq)
            V(lambda e: e.scalar_tensor_tensor(out=ysb[yb][:], in0=u[:, kq, :], scalar=dfm[:, kq:kq + 1], in1=ybank[:, ycols], op0=ALU.mult, op1=ALU.add),
              [un, yn, "B_d_fm"], [ynm])
            A(lambda e: e.activation(out=yt[yb][:], in_=ysb[yb][:], func=AF.Square, scale=SQ), [ynm], [ytn])
            A(lambda e: e.activation(out=yt[yb][:], in_=yt[yb][:], func=AF.Identity, bias=1.0, scale=1.0), [ytn], [ytn])
            G(lambda e: e.tensor_tensor(out=yt[yb][:], in0=yt[yb][:], in1=ysb[yb][:], op=ALU.mult), [ytn, ynm], [ytn])
            A(lambda e: e.activation(out=ysg[yb][:], in_=yt[yb][:], func=AF.Sigmoid, scale=1.5957691216057308), [ytn], [ysn])
            G(lambda e: e.tensor_tensor(out=gb[ch % 2][:, kq, :], in0=ysb[yb][:], in1=ysg[yb][:], op=ALU.mult), [ynm, ysn], [gbn])

        def emit_carry():
            V(lambda e: e.tensor_tensor(out=i1[:], in0=gre[:, :, CH - 1], in1=pc[:], op=ALU.mult), ["B_gre%d" % k for k in range(16)] + ["B_pc"], ["B_i1"])
            V(lambda e: e.tensor_tensor(out=i2[:], in0=gim[:, :, CH - 1], in1=psn[:], op=ALU.mult), ["B_gim%d" % k for k in range(16)] + ["B_psn"], ["B_i2"])
            V(lambda e: e.tensor_tensor(out=ini_re[:], in0=i1[:], in1=i2[:], op=ALU.subtract), ["B_i1", "B_i2"], ["B_inire"])
            V(lambda e: e.tensor_tensor(out=i1[:], in0=gre[:, :, CH - 1], in1=psn[:], op=ALU.mult), ["B_gre%d" % k for k in range(16)] + ["B_psn"], ["B_i1"])
            V(lambda e: e.tensor_tensor(out=i2[:], in0=gim[:, :, CH - 1], in1=pc[:], op=ALU.mult), ["B_gim%d" % k for k in range(16)] + ["B_pc"], ["B_i2"])
            V(lambda e: e.tensor_tensor(out=ini_im[:], in0=i1[:], in1=i2[:], op=ALU.add), ["B_i1", "B_i2"], ["B_iniim"])

        def emit_glu(ch):
            gcur = gb[ch % 2]
            gres = ["B_g%d_%d" % (ch % 2, kq) for kq in range(4)]
            for oc in range(4):
                zb = ps[6 + oc % 2]
                zn = "psZ%d" % (oc % 2)
                for kc in range(4):
                    P.op("pe", lambda e, zb=zb, oc=oc, kc=kc: e.matmul(zb[:, 0:CH], lhsT=wglu[:, kc, oc * 128:(oc + 1) * 128], rhs=gcur[:, kc, :],
                                                                      start=(kc == 0), stop=(kc == 3)),
                         reads=(gres + ["B_wglu"]) if kc == 0 else [], writes=[zn])
                A(lambda e, zb=zb, oc=oc: e.activation(out=zs[oc % 2][:], in_=zb[:, 0:CH], func=AF.Sigmoid, bias=bglu[:, oc:oc + 1], scale=1.0),
                  [zn, "B_bglu"], ["B_z%d" % (oc % 2)])
                G(lambda e, oc=oc: e.tensor_tensor(out=y2[ch % 2][:, oc, :], in0=gcur[:, oc, :], in1=zs[oc % 2][:], op=ALU.mult),
                  ["B_z%d" % (oc % 2), "B_g%d_%d" % (ch % 2, oc)], ["B_y2_%d" % (ch % 2)])
            P.dma("act", lambda e: e.dma_start(out=y2_v[:, :, ch * CH:(ch + 1) * CH], in_=y2[ch % 2][:]), "B_y2o%d" % (ch % 2),
                  reads=["B_y2_%d" % (ch % 2)])

        wst = [sb("B_wst%d" % i, [128, 4096], F32, st) for i in range(2)]
        wob = [sb("B_wob%d" % i, [128, 4096], BF16, st) for i in range(2)]
        wunits = [(e_, w_) for e_ in range(32) for w_ in range(3)]

        def w_load(ui):
            e_, w_ = wunits[ui]
            srcv = (w1v, w3v, w2v)[w_]
            P.dma("sp", lambda e: e.dma_start(out=wst[ui % 2][:], in_=srcv[e_ * 128:(e_ + 1) * 128, :]), "B_wst%d" % (ui % 2), writes=["B_wst%d" % (ui % 2)])

        def w_cast(ui):
            e_, w_ = wunits[ui]
            dstv = (w1b_d, w3b_d, w2b_d)[w_]
            si_, so_ = wst[ui % 2], wob[ui % 2]
            if w_ < 2:
                A(lambda e: e.copy(out=so_[:].rearrange("p (kc fc m) -> p kc fc m", kc=8, fc=4), in_=si_[:].rearrange("p (kc m fc) -> p kc fc m", kc=8, fc=4)),
                  ["B_wst%d" % (ui % 2)], ["B_wob%d" % (ui % 2)])
            else:
                A(lambda e: e.copy(out=so_[:], in_=si_[:]), ["B_wst%d" % (ui % 2)], ["B_wob%d" % (ui % 2)])
            P.dma("act", lambda e: e.dma_start(out=dstv[e_ * 128:(e_ + 1) * 128, :], in_=so_[:]), "B_wobo%d" % (ui % 2), reads=["B_wob%d" % (ui % 2)])

        jobs = [(ch, k) for ch in range(NCH) for k in range(16)]
        NJ = len(jobs)
        w_load(0)
        wnext = 0
        load_u(0)
        emit_Bu(0, 0)
        pend_epi = None
        for j, (ch, k) in enumerate(jobs):
            if k == 2 and ch + 1 < NCH:
                load_u(ch + 1)
            if j + 1 < NJ:
                emit_Bu(*jobs[j + 1])
            emit_mid(ch, k, j % 4)
            emit_C(ch, k, j % 4)
            if pend_epi is not None:
                emit_epi(*pend_epi)
                pend_epi = None
            if k % 4 == 3:
                pend_epi = (ch, k // 4)
            if k == 15:
                emit_carry()
            if k == 4 and ch >= 1:
                emit_glu(ch - 1)
            if j % 5 == 2 and wnext < len(wunits):
                if wnext + 1 < len(wunits):
                    w_load(wnext + 1)
                w_cast(wnext)
                wnext += 1
        emit_epi(*pend_epi)
        emit_glu(NCH - 1)
        while wnext < len(wunits):
            if wnext + 1 < len(wunits):
                w_load(wnext + 1)
            w_cast(wnext)
            wnext += 1
        P.barrier()
        P.emit()

    if dbg and "stopB" in dbg:
        es.close()
        return nc

    yaT_d = dscr("yaT", [4, 128, S], BF16)
    LT = 1152
    tab_d = dscr("tab", [8, LT], BF16)
    ind_d = din("ind", [32, S], BF16)
    ohp_d = din("ohp", [33, LT])
    rb33_d = din("rb33", [33, 8])
    rb31_d = din("rb31", [128, 8])
    emask_d = din("emask", [128, 2048])
    emaskm_d = din("emaskm", [128, 2048])
    own0_d = din("own0", [128, 2048])
    with ExitStack() as st:
        def V(fn, r, w):
            P.op("dve", fn, reads=r, writes=w)

        def A(fn, r, w):
            P.op("act", fn, reads=r, writes=w)

        def G(fn, r, w):
            P.op("pool", fn, reads=r, writes=w)

        PSN = lambda i: "PS%d" % i
        QA = [sb("C_QA%d" % i, [96, S], BF16, st) for i in range(2)]
        KA = [sb("C_KA%d" % i, [96, S], BF16, st) for i in range(2)]
        Vh = [sb("C_V%d" % i, [128, 64, 65], BF16, st) for i in range(2)]
        BT = [sb("C_BT%d" % i, [128, 5, 512], BF16, st) for i in range(2)]
        ohp = sb("C_ohp", [33, LT], F32, st)
        rb33 = sb("C_rb33", [33, 8], F32, st)
        ch = sb("C_ch", [128, 8], F32, st)
        tabs = sb("C_tabs", [8, LT], BF16, st)
        kmf = sb("C_kmf", [64, 32], F32, st)
        kmb = sb("C_kmb", [64, 32], BF16, st)
        emask = sb("C_emask", [128, 64, 32], F32, st)
        emaskm = sb("C_emaskm", [128, 64, 32], F32, st)
        own0 = sb("C_own0", [128, 64, 32], F32, st)
        Gt = sb("C_G", [128, 64, 32], F32, st)
        Tt = sb("C_T", [128, 64, 32], F32, st)
        mx = sb("C_mx", [128, 64, 8], F32, st)
        Mall = sb("C_Mall", [128, 64, 128], BF16, st)
        Pt = [sb("C_Pt%d" % i, [128, 512], BF16, st) for i in range(5)]
        rd = sb("C_rd", [65, 512], F32, st)
        onesf = sb("C_onesf", [65, 64], F32, st)
        bcs = sb("C_bcs", [64, 512], F32, st)
        yo = [sb("C_yo%d" % i, [64, 512], BF16, st) for i in range(2)]

        for i in range(2):
            P.dma("sp", lambda e, i=i: e.dma_start(out=KA[i][64:96, :], in_=ind_d), "C_ind%d" % i, writes=["C_KAind%d" % i])
            V(lambda e, i=i: e.memset(Vh[i][:, :, 64:65], 1.0), [], ["C_Vones%d" % i])
        P.dma("sp", lambda e: e.dma_start(out=ohp[:], in_=ohp_d), "C_ohp", writes=["C_ohp"])
        P.dma("sp", lambda e: e.dma_start(out=rb33[:], in_=rb33_d), "C_rb33", writes=["C_rb33"])
        P.dma("sp", lambda e: e.dma_start(out=ch[:], in_=rb31_d), "C_ch", writes=["C_ch"])
        P.dma("sp", lambda e: e.dma_start(out=emask[:].rearrange("p a b -> p (a b)"), in_=emask_d), "C_emask", writes=["C_emask"])
        P.dma("sp", lambda e: e.dma_start(out=emaskm[:].rearrange("p a b -> p (a b)"), in_=emaskm_d), "C_emaskm", writes=["C_emaskm"])
        P.dma("sp", lambda e: e.dma_start(out=own0[:].rearrange("p a b -> p (a b)"), in_=own0_d), "C_own0", writes=["C_own0"])
        V(lambda e: e.memset(onesf[:], 1.0), [], ["C_onesf"])
        G(lambda e: e.memset(Mall[:].rearrange("p a b -> p (a b)"), 0.0), [], ["C_Mall"])
        for c0 in range(0, LT, 384):
            P.op("pe", lambda e, c0=c0: e.matmul(ps[0][0:8, 0:384], lhsT=rb33[:, :], rhs=ohp[:, c0:c0 + 384], start=True, stop=True),
                 reads=["C_rb33", "C_ohp"], writes=[PSN(0)])
            V(lambda e, c0=c0: e.tensor_copy(out=tabs[:, c0:c0 + 384], in_=ps[0][0:8, 0:384]), [PSN(0)], ["C_tabs"])
        P.dma("sp", lambda e: e.dma_start(out=tab_d, in_=tabs[:]), "C_tabo", reads=["C_tabs"], writes=["C_tabd"])

        qT_v = qT_d.rearrange("k (a p) s -> (k a) p s", a=2)
        kT_v = kT_d.rearrange("k (a p) s -> (k a) p s", a=2)
        yaT_v = yaT_d.rearrange("k (a p) s -> (k a) p s", a=2)
        v_v = v_d.rearrange("(t p) (h d) -> h p t d", p=128, d=64)
        cnt = dict(s=0, o=0)

        def loads(h):
            b = h % 2
            P.dma("sp", lambda e: e.dma_start(out=QA[b][0:64, :], in_=qT_v[h]), "C_q%d" % b, writes=["C_QAq%d" % b])
            P.dma("sp", lambda e: e.dma_start(out=KA[b][0:64, :], in_=kT_v[h]), "C_k%d" % b, writes=["C_KAk%d" % b])
            P.dma("sp", lambda e: e.dma_start(out=Vh[b][:, :, 0:64], in_=v_v[h]), "C_v%d" % b, writes=["C_Vd%d" % b])
            for ri in range(5):
                rel = -128 + 128 * ri
                src_ap = bass.AP(tensor=tab_d.tensor, offset=h * LT + 385 - rel, ap=[[1, 128], [1, 512]])
                P.dma("sp", lambda e, ri=ri, src_ap=src_ap: e.dma_start(out=BT[b][:, ri, :], in_=src_ap), "C_bt%d" % b, reads=["C_tabd"], writes=["C_BT%d" % b])

        def gate_units(h):
            b = h % 2
            units = []

            def u_kmean():
                V(lambda e: e.tensor_reduce(out=kmf[:], in_=KA[b][0:64, :].rearrange("p (j n) -> p j n", n=256), op=ALU.add, axis=AX.X),
                  ["C_KAk%d" % b], ["C_kmf"])
                V(lambda e: e.tensor_scalar(out=kmb[:], in0=kmf[:], scalar1=1.0 / 256, scalar2=None, op0=ALU.mult), ["C_kmf"], ["C_kmb"])
            units.append(u_kmean)

            def mk_g1(g4):
                def u():
                    for q16 in range(16):
                        qt = g4 * 16 + q16
                        P.op("pe", lambda e, qt=qt, q16=q16: e.matmul(ps[7][:, q16 * 32:q16 * 32 + 32], lhsT=QA[b][0:64, qt * 128:(qt + 1) * 128], rhs=kmb[:, :],
                                                                     start=True, stop=True),
                             reads=["C_QAq%d" % b, "C_kmb"], writes=[PSN(7)])
                    V(lambda e: e.tensor_tensor(out=Gt[:, g4 * 16:(g4 + 1) * 16, :].rearrange("p a b -> p (a b)"), in0=ps[7][:, :],
                                                in1=emask[:, g4 * 16:(g4 + 1) * 16, :].rearrange("p a b -> p (a b)"), op=ALU.add),
                      [PSN(7), "C_emask"], ["C_G"])
                return u
            for g4 in range(4):
                units.append(mk_g1(g4))

            def u_sel():
                for qt in range(64):
                    V(lambda e, qt=qt: e.max(out=mx[:, qt, :], in_=Gt[:, qt, :]), ["C_G"], ["C_mx%d" % qt])
                V(lambda e: e.tensor_tensor(out=Tt[:], in0=Gt[:], in1=mx[:, :, 2:3].to_broadcast([128, 64, 32]), op=ALU.is_ge),
                  ["C_G"] + ["C_mx%d" % qt for qt in range(64)], ["C_T"])
                V(lambda e: e.scalar_tensor_tensor(out=Tt[:].rearrange("p a b -> p (a b)"), in0=Tt[:].rearrange("p a b -> p (a b)"), scalar=-NEG,
                                                   in1=emaskm[:].rearrange("p a b -> p (a b)"), op0=ALU.mult, op1=ALU.add), ["C_T", "C_emaskm"], ["C_T"])
                V(lambda e: e.tensor_tensor(out=Tt[:].rearrange("p a b -> p (a b)"), in0=Tt[:].rearrange("p a b -> p (a b)"),
                                            in1=own0[:].rearrange("p a b -> p (a b)"), op=ALU.max), ["C_T", "C_own0"], ["C_T"])
                V(lambda e: e.tensor_scalar(out=Mall[:, :, 64:96], in0=Tt[:], scalar1=ch[:, h:h + 1], scalar2=None, op0=ALU.add), ["C_T", "C_ch"], ["C_Mall"])
            units.append(u_sel)

            def mk_g2(g):
                def u():
                    for i4 in range(4):
                        qt = g * 4 + i4
                        P.op("pe", lambda e, qt=qt, i4=i4: e.matmul(ps[7][:, i4 * 128:(i4 + 1) * 128], lhsT=Mall[:, qt, :], rhs=identb[:, :], start=True, stop=True),
                             reads=["C_Mall", "identb"], writes=[PSN(7)])
                    V(lambda e: e.tensor_copy(out=QA[b][64:96, g * 512:(g + 1) * 512], in_=ps[7][64:96, :]), [PSN(7)], ["C_QAm%d" % b])
                return u
            for g in range(16):
                units.append(mk_g2(g))
            return units

        SB = [0, 1, 2, 3, 4]
        LOOK = 4
        FDELAY = 16

        def main(h, side_units):
            b = h % 2
            qres = ["C_QAq%d" % b, "C_QAm%d" % b]
            kres = ["C_KAk%d" % b, "C_KAind%d" % b]
            jobs = [(sbk, kt, 4 * (sbk + 1)) for sbk in range(16) for kt in range(4 * (sbk + 1))]
            slot = {}
            pend = []

            def emit_S(i):
                sbk, kt, nkt = jobs[i]
                qc = slice(sbk * 512, (sbk + 1) * 512)
                si = cnt["s"] % len(SB)
                cnt["s"] += 1
                slot[i] = si
                sbi = SB[si]
                sbn = ps[sbi]
                pt = Pt[si]
                pn = "C_Pt%d" % si
                rel = kt * 128 - sbk * 512
                near = rel >= -128
                P.op("pe", lambda e: e.matmul(sbn[:, :], lhsT=KA[b][:, kt * 128:(kt + 1) * 128], rhs=QA[b][:, qc], start=True, stop=not near),
                     reads=qres + kres, writes=[PSN(sbi)])
                if near:
                    ri = (rel + 128) // 128
                    P.op("pe", lambda e: e.matmul(sbn[:, :], lhsT=identr[:, :], rhs=BT[b][:, ri, :], start=False, stop=True),
                         reads=["C_BT%d" % b, "identr"], writes=[PSN(sbi)])
                A(lambda e: e.activation(out=pt[:], in_=sbn[:, :], func=AF.Exp), [PSN(sbi)], [pn])

            def emit_fin(sbk):
                qc = slice(sbk * 512, (sbk + 1) * 512)
                obi = 5 + sbk % 2
                ob = ps[obi]
                V(lambda e: e.reciprocal(out=rd[64:65, :], in_=ob[64:65, :]), [PSN(obi)], ["C_rd"])
                P.op("pe", lambda e: e.matmul(ps[7][0:64, :], lhsT=onesf[64:65, :], rhs=rd[64:65, :], start=True, stop=True),
                     reads=["C_rd", "C_onesf"], writes=[PSN(7)])
                V(lambda e: e.tensor_copy(out=bcs[:], in_=ps[7][0:64, :]), [PSN(7)], ["C_bcs"])
                yb = yo[sbk % 2]
                yn = "C_yo%d" % (sbk % 2)
                V(lambda e: e.tensor_tensor(out=yb[:], in0=ob[0:64, :], in1=bcs[:], op=ALU.mult), [PSN(obi), "C_bcs"], [yn])
                P.dma("pool", lambda e: e.dma_start(out=yaT_v[h][:, qc], in_=yb[:]), yn + "o", reads=[yn])

            def emit_PV(i):
                sbk, kt, nkt = jobs[i]
                if kt == 0:
                    while pend and pend[0][1] <= sbk - 2:
                        emit_fin(pend.pop(0)[1])
                si = slot.pop(i)
                pt = Pt[si]
                pn = "C_Pt%d" % si
                obi = 5 + sbk % 2
                ob = ps[obi]
                P.op("pe", lambda e: e.matmul(ob[0:65, :], lhsT=Vh[b][:, kt, :], rhs=pt[:], start=(kt == 0), stop=(kt == nkt - 1)),
                     reads=[pn, "C_Vd%d" % b, "C_Vones%d" % b], writes=[PSN(obi)])
                if kt == nkt - 1:
                    pend.append((i + FDELAY, sbk))

            NJ = len(jobs)
            nu = len(side_units)
            ustep = max(1, (NJ - 40) // (nu + 1)) if nu else 0
            ui = 0
            for i in range(NJ + LOOK):
                if i < NJ:
                    emit_S(i)
                if i >= LOOK:
                    emit_PV(i - LOOK)
                while pend and pend[0][0] <= i:
                    emit_fin(pend.pop(0)[1])
                if nu and ui < nu and i >= 20 and (i - 20) % ustep == 0:
                    side_units[ui]()
                    ui += 1
            while pend:
                emit_fin(pend.pop(0)[1])
            while ui < nu:
                side_units[ui]()
                ui += 1

        zt = sb("C_zt", [128, 4096], BF16, st)
        G(lambda e: e.memset(zt[:], 0.0), [], ["C_zt"])
        loads(0)
        for u in gate_units(0):
            u()
        for h in range(8):
            if h + 1 < 8:
                loads(h + 1)
            for zi in range(8 * h, 8 * h + 8):
                P.dma("sp", lambda e, zi=zi: e.dma_start(out=Xs_d[zi * 512:(zi + 1) * 512, :].rearrange("(p r) d -> p (r d)", r=4), in_=zt[:]), "C_zfill", reads=["C_zt"])
            main(h, gate_units(h + 1) if h + 1 < 8 else [])
        P.barrier()
        P.emit()

    if dbg and "stopC" in dbg:
        es.close()
        return nc

    x1_d = dscr("x1", [S, D], F32)
    wups_d = din("w_up_ssm", [512, 1024]); wupa_d = din("w_up_attn", [512, 1024]); wout_d = din("w_out", [1024, 1024])
    g2bc_d = din("g2bc", [128, 1024]); gfbc_d = din("gfbc", [128, 1024])
    wr_d = din("wr", [1024, 36]); bias36_d = din("bias36", [128, 36])
    identf_d = din("identf", [128, 128])
    M1 = sb("M1", [128, 64, 32], F32)
    M2 = sb("M2", [128, 64, 32], F32)
    cw1 = sb("cw1", [128, 64], F32)
    cw2 = sb("cw2", [128, 64], F32)
    xn2p_d = dscr("xn2p", [S, D], BF16)
    with ExitStack() as st:
        def V(fn, r, w):
            P.op("dve", fn, reads=r, writes=w)

        def A(fn, r, w):
            P.op("act", fn, reads=r, writes=w)

        def G(fn, r, w):
            P.op("pool", fn, reads=r, writes=w)

        wups = sb("D_wups", [128, 4, 1024], BF16, st)
        wupa = sb("D_wupa", [128, 4, 1024], BF16, st)
        woutb = sb("D_wout", [128, 8, 1024], BF16, st)
        stage = sb("D_stage", [128, 4096], F32, st)
        g2bc = sb("D_g2bc", [128, 1024], F32, st)
        wr = sb("D_wr", [128, 8, 36], F32, st)
        bias36 = sb("D_bias36", [128, 36], F32, st)
        identf = sb("D_identf", [128, 128], F32, st)
        P.dma("sp", lambda e: e.dma_start(out=g2bc[:], in_=g2bc_d), "D_g2", writes=["D_g2bc"])
        P.dma("sp", lambda e: e.dma_start(out=wr[:], in_=wr_d.rearrange("(k p) n -> p k n", p=128)), "D_wr", writes=["D_wr"])
        P.dma("sp", lambda e: e.dma_start(out=bias36[:], in_=bias36_d), "D_b36", writes=["D_bias36"])
        P.dma("sp", lambda e: e.dma_start(out=identf[:], in_=identf_d), "D_idf", writes=["D_identf"])
        for (wd, wt, nk, nm) in ((wups_d, wups, 4, "D_wups"), (wupa_d, wupa, 4, "D_wupa"), (wout_d[0:512, :], woutb, 4, "D_wout0"), (wout_d[512:1024, :], woutb, 4, "D_wout1")):
            koff = 4 if nm == "D_wout1" else 0
            P.dma("sp", lambda e, wd=wd: e.dma_start(out=stage[:].rearrange("p (k n) -> p k n", k=4), in_=wd.rearrange("(k p) n -> p k n", p=128)),
                  "D_stage", writes=["D_stage"])
            V(lambda e, wt=wt, koff=koff: e.tensor_copy(out=wt[:, koff:koff + 4, :].rearrange("p k n -> p (k n)"), in_=stage[:]), ["D_stage"], [nm])
        wres = ["D_wups", "D_wupa", "D_wout0", "D_wout1"]

        y2t = [sb("D_y2t%d" % i, [128, 4, 512], BF16, st) for i in range(2)]
        yat = [sb("D_yat%d" % i, [128, 4, 512], BF16, st) for i in range(2)]
        gt = sb("D_gt", [128, 16, 512], BF16, st)
        m1 = [sb("D_m1_%d" % i, [128, 512], F32, st) for i in range(2)]
        m2 = [sb("D_m2_%d" % i, [128, 512], F32, st) for i in range(2)]
        mT = sb("D_mT", [128, 8, 512], BF16, st)
        xs = sb("D_xs", [128, 4, 1024], F32, st)
        x1s = sb("D_x1s", [128, 4, 1024], F32, st)
        junk = sb("D_junk", [128, 1024], BF16, st)
        ss = sb("D_ss", [128, 4], F32, st)
        rstd = sb("D_rstd", [128, 4], F32, st)
        xn2f = [sb("D_xn2f%d" % i, [128, 1024], F32, st) for i in range(4)]
        xn2b = sb("D_xn2b", [128, 4, 1024], BF16, st)
        xT32 = [sb("D_xT32_%d" % i, [128, 8, 128], F32, st) for i in range(2)]
        L = sb("D_L", [128, 36], F32, st)
        sm = sb("D_sm", [128, 16], F32, st)
        ohg = sb("D_ohg", [128, 4], F32, st)
        msk = sb("D_msk", [128, 32], F32, st)
        mx = sb("D_mx", [128, 8], F32, st)
        ta = sb("D_ta", [128, 32], F32, st)
        tb_ = sb("D_tb", [128, 32], F32, st)
        y2_v = y2T_d.rearrange("k p s -> p k s")
        ya_v = yaT_d.rearrange("k p s -> p k s")
        gate_v = gate_d.rearrange("k p s -> p k s")
        x_t = x_d.rearrange("(t j p) d -> t p j d", j=4, p=128)
        x1_t = x1_d.rearrange("(t j p) d -> t p j d", j=4, p=128)
        xn2p_t = xn2p_d.rearrange("(t j p) d -> t p j d", j=4, p=128)

        def loads(T):
            cs = slice(T * 512, (T + 1) * 512)
            P.dma("sp", lambda e, T=T, cs=cs: e.dma_start(out=y2t[T % 2][:], in_=y2_v[:, :, cs]), "D_y2t%d" % (T % 2), writes=["D_y2t%d" % (T % 2)])
            P.dma("sp", lambda e, T=T, cs=cs: e.dma_start(out=yat[T % 2][:], in_=ya_v[:, :, cs]), "D_yat%d" % (T % 2), writes=["D_yat%d" % (T % 2)])

        loads(0)
        ev = 0
        for T in range(NT):
            cs = slice(T * 512, (T + 1) * 512)
            P.dma("sp", lambda e, cs=cs: e.dma_start(out=gt[:], in_=gate_v[:, :, cs]), "D_gt", writes=["D_gt"])
            P.dma("sp", lambda e, T=T: e.dma_start(out=xs[:], in_=x_t[T]), "D_xs", writes=["D_xs"])
            if T + 1 < NT:
                loads(T + 1)
            yS, yA = y2t[T % 2], yat[T % 2]
            for fc in range(8):
                bS = ps[(2 * ev) % 4]
                bA = ps[(2 * ev + 1) % 4]
                nS, nA = "psD%d" % ((2 * ev) % 4), "psD%d" % ((2 * ev + 1) % 4)
                for kc in range(4):
                    P.op("pe", lambda e, bS=bS, kc=kc, fc=fc, yS=yS: e.matmul(bS[:, :], lhsT=wups[:, kc, fc * 128:(fc + 1) * 128], rhs=yS[:, kc, :],
                                                                             start=(kc == 0), stop=(kc == 3)),
                         reads=(wres + ["D_y2t%d" % (T % 2)]) if kc == 0 else [], writes=[nS])
                for kc in range(4):
                    P.op("pe", lambda e, bA=bA, kc=kc, fc=fc, yA=yA: e.matmul(bA[:, :], lhsT=wupa[:, kc, fc * 128:(fc + 1) * 128], rhs=yA[:, kc, :],
                                                                             start=(kc == 0), stop=(kc == 3)),
                         reads=(wres + ["D_yat%d" % (T % 2)]) if kc == 0 else [], writes=[nA])
                b = ev % 2
                V(lambda e, bS=bS, fc=fc, b=b: e.tensor_tensor(out=m1[b][:], in0=bS[:, :], in1=gt[:, fc, :], op=ALU.mult), [nS, "D_gt"], ["D_m1_%d" % b])
                V(lambda e, bA=bA, fc=fc, b=b: e.tensor_tensor(out=m2[b][:], in0=bA[:, :], in1=gt[:, 8 + fc, :], op=ALU.mult), [nA, "D_gt"], ["D_m2_%d" % b])
                G(lambda e, fc=fc, b=b: e.tensor_tensor(out=mT[:, fc, :], in0=m1[b][:], in1=m2[b][:], op=ALU.add), ["D_m1_%d" % b, "D_m2_%d" % b], ["D_mT%d" % fc])
                ev += 1
            mres = ["D_mT%d" % fc for fc in range(8)]
            for j in range(4):
                for hf in range(2):
                    bk = ps[4 + hf]
                    bn = "psE%d" % hf
                    for kc in range(8):
                        P.op("pe", lambda e, bk=bk, kc=kc, j=j, hf=hf: e.matmul(bk[:, :], lhsT=mT[:, kc, j * 128:(j + 1) * 128], rhs=woutb[:, kc, hf * 512:(hf + 1) * 512],
                                                                               start=(kc == 0), stop=(kc == 7)),
                             reads=(mres + wres) if kc == 0 else [], writes=[bn])
                    V(lambda e, bk=bk, j=j, hf=hf: e.tensor_tensor(out=x1s[:, j, hf * 512:(hf + 1) * 512], in0=bk[:, :], in1=xs[:, j, hf * 512:(hf + 1) * 512], op=ALU.add),
                      [bn, "D_xs"], ["D_x1s%d" % j])
                A(lambda e, j=j: e.activation(out=junk[:], in_=x1s[:, j, :], func=AF.Square, accum_out=ss[:, j:j + 1]), ["D_x1s%d" % j], ["D_junk", "D_ss%d" % j])
            P.dma("act", lambda e, T=T: e.dma_start(out=x1_t[T], in_=x1s[:]), "D_x1o", reads=["D_x1s%d" % j for j in range(4)])
            A(lambda e: e.activation(out=rstd[:], in_=ss[:], func=AF.Sqrt, scale=1.0 / D, bias=EPS), ["D_ss%d" % j for j in range(4)], ["D_rstd"])
            V(lambda e: e.reciprocal(out=rstd[:], in_=rstd[:]), ["D_rstd"], ["D_rstd"])
            for j in range(4):
                xf = xn2f[j]
                xfn = "D_xn2f%d" % j
                V(lambda e, j=j, xf=xf: e.scalar_tensor_tensor(out=xf[:], in0=x1s[:, j, :], scalar=rstd[:, j:j + 1], in1=g2bc[:], op0=ALU.mult, op1=ALU.mult),
                  ["D_x1s%d" % j, "D_rstd", "D_g2bc"], [xfn])

            def tbanks(j):
                if j % 2 == 0:
                    return (ps[6], "psF0"), (ps[7], "psF1"), (ps[2], "psD2")
                return (ps[0], "psD0"), (ps[1], "psD1"), (ps[3], "psD3")

            def r_trans(j):
                xf = xn2f[j]
                xfn = "D_xn2f%d" % j
                (ba, bna), (bb, bnb), _ = tbanks(j)
                x32 = xT32[j % 2]
                for kc in range(8):
                    bk, bn_ = (ba, bna) if kc < 4 else (bb, bnb)
                    P.op("pe", lambda e, kc=kc, bk=bk: e.transpose(out=bk[:, (kc % 4) * 128:(kc % 4 + 1) * 128], in_=xf[:, kc * 128:(kc + 1) * 128], identity=identf[:]),
                         reads=[xfn, "D_identf"], writes=[bn_])
                A(lambda e: e.copy(out=x32[:, 0:4, :].rearrange("p k n -> p (k n)"), in_=ba[:, :]), [bna], ["D_xT32a%d" % (j % 2)])
                A(lambda e: e.copy(out=x32[:, 4:8, :].rearrange("p k n -> p (k n)"), in_=bb[:, :]), [bnb], ["D_xT32b%d" % (j % 2)])

            def r_logits(j):
                _, _, (br, brn) = tbanks(j)
                x32 = xT32[j % 2]
                for kc in range(8):
                    P.op("pe", lambda e, kc=kc: e.matmul(br[:, 0:36], lhsT=x32[:, kc, :], rhs=wr[:, kc, :], start=(kc == 0), stop=(kc == 7)),
                         reads=["D_xT32a%d" % (j % 2), "D_xT32b%d" % (j % 2), "D_wr"] if kc == 0 else [], writes=[brn])

            r_trans(0)
            r_trans(1)
            for j in range(4):
                r_logits(j)
                if j + 2 < 4:
                    r_trans(j + 2)
                _, _, (br, brn) = tbanks(j)
                idx = T * 4 + j
                V(lambda e, br=br: e.tensor_tensor(out=L[:], in0=br[:, 0:36], in1=bias36[:], op=ALU.add), [brn, "D_bias36"], ["D_L"])
                V(lambda e: e.tensor_reduce(out=sm[:, 0:1], in_=L[:, 0:4], op=ALU.max, axis=AX.X), ["D_L"], ["D_sm"])
                V(lambda e: e.tensor_scalar(out=ohg[:], in0=L[:, 0:4], scalar1=sm[:, 0:1], scalar2=None, op0=ALU.is_ge), ["D_L", "D_sm"], ["D_ohg"])
                V(lambda e: e.tensor_scalar(out=sm[:, 1:2], in0=sm[:, 0:1], scalar1=-1.0, scalar2=None, op0=ALU.mult), ["D_sm"], ["D_sm"])
                A(lambda e: e.activation(out=ta[:, 0:4], in_=L[:, 0:4], func=AF.Exp, bias=sm[:, 1:2], scale=1.0, accum_out=sm[:, 2:3]), ["D_L", "D_sm"], ["D_ta", "D_sm"])
                V(lambda e: e.reciprocal(out=sm[:, 3:4], in_=sm[:, 2:3]), ["D_sm"], ["D_sm"])
                V(lambda e: e.tensor_scalar(out=ohg[:], in0=ohg[:], scalar1=1e30, scalar2=-1e30, op0=ALU.mult, op1=ALU.add), ["D_ohg"], ["D_ohg"])
                V(lambda e: e.tensor_tensor(out=msk[:].rearrange("p (g n) -> p g n", g=4), in0=L[:, 4:36].rearrange("p (g n) -> p g n", g=4),
                                            in1=ohg[:, :].unsqueeze(2).to_broadcast([128, 4, 8]), op=ALU.add), ["D_L", "D_ohg"], ["D_msk"])
                V(lambda e: e.max(out=mx[:], in_=msk[:]), ["D_msk"], ["D_mx"])
                V(lambda e: e.tensor_tensor(out=sm[:, 4:5], in0=mx[:, 1:2], in1=mx[:, 0:1], op=ALU.subtract), ["D_mx"], ["D_sm"])
                A(lambda e: e.activation(out=sm[:, 5:6], in_=sm[:, 4:5], func=AF.Exp), ["D_sm"], ["D_sm"])
                V(lambda e: e.tensor_scalar(out=sm[:, 6:7], in0=sm[:, 5:6], scalar1=1.0, scalar2=None, op0=ALU.add), ["D_sm"], ["D_sm"])
                V(lambda e: e.reciprocal(out=sm[:, 6:7], in_=sm[:, 6:7]), ["D_sm"], ["D_sm"])
                V(lambda e: e.tensor_tensor(out=sm[:, 7:8], in0=sm[:, 5:6], in1=sm[:, 6:7], op=ALU.mult), ["D_sm"], ["D_sm"])
                V(lambda e, idx=idx: e.tensor_tensor(out=cw1[:, idx:idx + 1], in0=sm[:, 6:7], in1=sm[:, 3:4], op=ALU.mult), ["D_sm"], ["cw1"])
                V(lambda e, idx=idx: e.tensor_tensor(out=cw2[:, idx:idx + 1], in0=sm[:, 7:8], in1=sm[:, 3:4], op=ALU.mult), ["D_sm"], ["cw2"])
                V(lambda e, idx=idx: e.tensor_scalar(out=M1[:, idx, :], in0=msk[:], scalar1=mx[:, 0:1], scalar2=None, op0=ALU.is_equal), ["D_msk", "D_mx"], ["M1"])
                V(lambda e, idx=idx: e.tensor_scalar(out=M2[:, idx, :], in0=msk[:], scalar1=mx[:, 1:2], scalar2=None, op0=ALU.is_equal), ["D_msk", "D_mx"], ["M2"])
            for j in range(4):
                A(lambda e, j=j: e.copy(out=xn2b[:, j, :].rearrange("q (kc p) -> q kc p", kc=8), in_=xn2f[j][:].rearrange("q (p kc) -> q kc p", kc=8)),
                  ["D_xn2f%d" % j], ["D_xn2b%d" % j])
            P.dma("pool", lambda e, T=T: e.dma_start(out=xn2p_t[T], in_=xn2b[:]), "D_xnTo", reads=["D_xn2b%d" % j for j in range(4)])
        P.barrier()
        P.emit()

    if dbg and "stopD" in dbg:
        es.close()
        return nc

    NTL = 64
    NSL = NTL * 512
    Ys_d = dscr("Ys", [NSL, D], BF16)
    triu_d = din("triu", [128, 128], BF16)
    onesb_d = din("onesb", [128, 128], BF16)
    m512_d = din("m512", [128, 1024])
    t512_d = din("t512", [128, 2048])
    pcol_d = din("pcol", [128, 1])
    IOA = bass.IndirectOffsetOnAxis
    with ExitStack() as st:
        def V(fn, r, w):
            P.op("dve", fn, reads=r, writes=w)

        def A(fn, r, w):
            P.op("act", fn, reads=r, writes=w)

        def G(fn, r, w):
            P.op("pool", fn, reads=r, writes=w)

        S1i = sb("E_S1i", [128, 64], I32, st)
        S2i = sb("E_S2i", [128, 64], I32, st)
        WIDX = sb("E_widx", [128, 64], I32, st)
        gfbc = sb("E_gfbc", [128, 1024], F32, st)
        P.dma("sp", lambda e: e.dma_start(out=gfbc[:], in_=gfbc_d), "E_gf", writes=["E_gfbc"])
        xn2p_t = xn2p_d.rearrange("(t j p) d -> t p j d", j=4, p=128)
        fl3 = lambda t: t[:].rearrange("p a b -> p (a b)")

        with ExitStack() as sp:
            OHb = sb("E_OHb", [128, 2048], BF16, sp)
            triu = sb("E_triu", [128, 128], BF16, sp)
            onesb = sb("E_onesb", [128, 128], BF16, sp)
            TOT = sb("E_TOT", [128, 64, 32], F32, sp)
            XA = sb("E_XA", [128, 64, 32], F32, sp)
            XB = sb("E_XB", [128, 64, 32], F32, sp)
            SL = sb("E_SL", [128, 64, 32], F32, sp)
            tmp = sb("E_tmp", [128, 64, 32], F32, sp)
            m512 = sb("E_m512", [128, 32, 32], F32, sp)
            t512 = sb("E_t512", [128, 32, 64], F32, sp)
            cmp1 = sb("E_cmp1", [128, 32, 32], F32, sp)
            cmp2 = sb("E_cmp2", [128, 32, 64], F32, sp)
            pcol = sb("E_pcol", [128, 1], F32, sp)
            CE = sb("E_CE", [128, 32], F32, sp)
            Pc = sb("E_Pc", [128, 32], F32, sp)
            BEND = sb("E_BEND", [128, 32], F32, sp)
            BASE = sb("E_BASE", [128, 32], F32, sp)
            ones32 = sb("E_ones32", [128, 32], F32, sp)
            zc = sb("E_zc", [128, 1], F32, sp)
            te = sb("E_te", [128, 64], F32, sp)
            wf = sb("E_wf", [128, 64], F32, sp)
            s1f = sb("E_s1f", [128, 64], F32, sp)
            s2f = sb("E_s2f", [128, 64], F32, sp)
            xq = [sb("E_pxq%d" % i, [128, 4, 1024], BF16, sp) for i in range(2)]
            P.dma("sp", lambda e: e.dma_start(out=triu[:], in_=triu_d), "E_triu", writes=["E_triu"])
            P.dma("sp", lambda e: e.dma_start(out=onesb[:], in_=onesb_d), "E_onesb", writes=["E_onesb"])
            P.dma("sp", lambda e: e.dma_start(out=fl3(m512), in_=m512_d), "E_m512", writes=["E_m512"])
            P.dma("sp", lambda e: e.dma_start(out=fl3(t512), in_=t512_d), "E_t512", writes=["E_t512"])
            P.dma("sp", lambda e: e.dma_start(out=pcol[:], in_=pcol_d), "E_pcol", writes=["E_pcol"])
            V(lambda e: e.memset(ones32[:], 1.0), [], ["E_ones32"])
            V(lambda e: e.memset(zc[:], 0.0), [], ["E_zc"])
            V(lambda e: e.tensor_tensor(out=OHb[:], in0=fl3(M1), in1=fl3(M2), op=ALU.add), ["M1", "M2"], ["E_OHb"])
            for c in range(4):
                P.op("pe", lambda e, c=c: e.matmul(ps[c][:, :], lhsT=triu[:, :], rhs=OHb[:, c * 512:(c + 1) * 512], start=True, stop=True),
                     reads=["E_triu", "E_OHb"], writes=["psR%d" % c])
                P.op("pe", lambda e, c=c: e.matmul(ps[4 + c][:, :], lhsT=onesb[:, :], rhs=OHb[:, c * 512:(c + 1) * 512], start=True, stop=True),
                     reads=["E_onesb", "E_OHb"], writes=["psT%d" % c])
                A(lambda e, c=c: e.copy(out=fl3(TOT)[:, c * 512:(c + 1) * 512], in_=ps[4 + c][:, :]), ["psT%d" % c], ["E_TOT"])
            cur, curn = TOT, ["E_TOT"]
            bufs = [(XA, "E_XA"), (XB, "E_XB")]
            d = 1
            k = 0
            while d < 64:
                nxt, nxtn = bufs[k % 2]
                w = d * 32
                V(lambda e, cur=cur, nxt=nxt, w=w: e.tensor_tensor(out=fl3(nxt)[:, w:2048], in0=fl3(cur)[:, w:2048], in1=fl3(cur)[:, 0:2048 - w], op=ALU.add),
                  curn, [nxtn])
                A(lambda e, cur=cur, nxt=nxt, w=w: e.copy(out=fl3(nxt)[:, 0:w], in_=fl3(cur)[:, 0:w]), curn, [nxtn + "h"])
                cur, curn = nxt, [nxtn, nxtn + "h"]
                d *= 2
                k += 1
            INC, INCn = cur, curn
            EXC, EXCn = XA, "E_XA"
            V(lambda e: e.tensor_tensor(out=fl3(EXC), in0=fl3(INC), in1=fl3(TOT), op=ALU.subtract), INCn + ["E_TOT"], [EXCn, "E_XAh"])
            Cn = INC[:, 63, :]
            V(lambda e: e.tensor_tensor(out=cmp1[:], in0=Cn.unsqueeze(2).to_broadcast([128, 32, 32]), in1=m512[:], op=ALU.is_gt), INCn + ["E_m512"], ["E_cmp1"])
            V(lambda e: e.tensor_reduce(out=CE[:], in_=cmp1[:], op=ALU.add, axis=AX.X), ["E_cmp1"], ["E_CE"])
            V(lambda e: e.tensor_scalar(out=Pc[:], in0=CE[:], scalar1=512.0, scalar2=None, op0=ALU.mult), ["E_CE"], ["E_Pc"])
            V(lambda e: e.tensor_tensor_scan(out=BEND[:], data0=ones32[:], data1=Pc[:], initial=zc[:, 0:1], op0=ALU.mult, op1=ALU.add),
              ["E_Pc", "E_ones32", "E_zc"], ["E_BEND"])
            V(lambda e: e.tensor_tensor(out=BASE[:], in0=BEND[:], in1=Pc[:], op=ALU.subtract), ["E_BEND", "E_Pc"], ["E_BASE"])
            V(lambda e: e.tensor_tensor(out=cmp2[:], in0=BEND[:, :].unsqueeze(2).to_broadcast([128, 32, 64]), in1=t512[:], op=ALU.is_le), ["E_BEND", "E_t512"], ["E_cmp2"])
            V(lambda e: e.tensor_reduce(out=te[:], in_=cmp2[:].rearrange("p e t -> p t e"), op=ALU.add, axis=AX.X), ["E_cmp2"], ["E_te"])
            V(lambda e: e.tensor_scalar(out=wf[:], in0=te[:], scalar1=128.0, scalar2=pcol[:, 0:1], op0=ALU.mult, op1=ALU.add), ["E_te", "E_pcol"], ["E_wf"])
            V(lambda e: e.tensor_copy(out=WIDX[:], in_=wf[:]), ["E_wf"], ["E_widx"])
            for c in range(4):
                V(lambda e, c=c: e.tensor_tensor(out=fl3(SL)[:, c * 512:(c + 1) * 512], in0=ps[c][:, :], in1=fl3(EXC)[:, c * 512:(c + 1) * 512], op=ALU.add),
                  ["psR%d" % c, EXCn], ["E_SL"])
            V(lambda e: e.tensor_tensor(out=SL[:].rearrange("p t e -> p e t"), in0=SL[:].rearrange("p t e -> p e t"),
                                        in1=BASE[:, :].unsqueeze(2).to_broadcast([128, 32, 64]), op=ALU.add), ["E_SL", "E_BASE"], ["E_SL"])
            V(lambda e: e.tensor_tensor(out=fl3(tmp), in0=fl3(M1), in1=fl3(SL), op=ALU.mult), ["M1", "E_SL"], ["E_tmp"])
            V(lambda e: e.tensor_reduce(out=s1f[:], in_=tmp[:], op=ALU.add, axis=AX.X), ["E_tmp"], ["E_s1f"])
            V(lambda e: e.tensor_copy(out=S1i[:], in_=s1f[:]), ["E_s1f"], ["E_S1i"])
            V(lambda e: e.tensor_tensor(out=fl3(tmp), in0=fl3(M2), in1=fl3(SL), op=ALU.mult), ["M2", "E_SL", "E_s1f"], ["E_tmp"])
            V(lambda e: e.tensor_reduce(out=s2f[:], in_=tmp[:], op=ALU.add, axis=AX.X), ["E_tmp"], ["E_s2f"])
            V(lambda e: e.tensor_copy(out=S2i[:], in_=s2f[:]), ["E_s2f"], ["E_S2i"])
            if dbg and "dumpE" in dbg:
                for nm_, t_ in (("dbg_S1i", S1i), ("dbg_S2i", S2i), ("dbg_widx", WIDX)):
                    dd = nc.dram_tensor(nm_, [128, 64], I32, kind="ExternalOutput").ap()
                    P.dma("sp", lambda e, dd=dd, t_=t_: e.dma_start(out=dd, in_=t_[:]), nm_, reads=["E_S1i", "E_S2i", "E_widx"])
            for T in range(NT):
                xb_ = xq[T % 2]
                xbn = "E_pxq%d" % (T % 2)
                P.dma("sp", lambda e, T=T, xb_=xb_: e.dma_start(out=xb_[:], in_=xn2p_t[T]), xbn, writes=[xbn])
                for j in range(4):
                    idx = T * 4 + j
                    for (Si, Sn) in ((S1i, "E_S1i"), (S2i, "E_S2i")):
                        P.dma("pool", lambda e, xb_=xb_, j=j, idx=idx, Si=Si: e.indirect_dma_start(
                            out=Xs_d[:, :], out_offset=IOA(ap=Si[:, idx:idx + 1], axis=0), in_=xb_[:, j, :], in_offset=None),
                            "E_sc%d" % (T % 2), reads=[xbn, Sn])
            P.barrier()
            P.emit()

        with ExitStack() as sx:
            NWB = 3
            w1b = [sb("E_w1b%d" % i, [128, 8, 4, 128], BF16, sx) for i in range(NWB)]
            w3b = [sb("E_w3b%d" % i, [128, 8, 4, 128], BF16, sx) for i in range(NWB)]
            w2b = [sb("E_w2b%d" % i, [128, 4, 1024], BF16, sx) for i in range(NWB)]
            xs_ = [sb("E_xs%d" % i, [128, 4, 1024], BF16, sx) for i in range(2)]
            xT = [sb("E_xT%d" % i, [128, 8, 512], BF16, sx) for i in range(2)]
            hidT = sb("E_hidT", [128, 4, 512], BF16, sx)
            sil = [sb("E_sil%d" % i, [128, 512], F32, sx) for i in range(2)]
            yq = [sb("E_yq%d" % i, [128, 4, 1024], BF16, sx) for i in range(2)]
            Xs_t = Xs_d.rearrange("(t j p) d -> t p j d", j=4, p=128)
            Ys_t = Ys_d.rearrange("(t j p) d -> t p j d", j=4, p=128)

            bcreg = {}

            def wfetch(e, dst, wv, t):
                if "r" not in bcreg:
                    bcreg["r"] = nc.alloc_register(mybir.EngineType.Pool, "wbound")
                    e.reg_mov(bcreg["r"], 4095)
                return e.indirect_dma_start(out=dst, out_offset=None, in_=wv[:, :], in_offset=IOA(ap=WIDX[:, t:t + 1], axis=0),
                                            bounds_check=bcreg["r"], oob_is_err=False)

            def fetch(t):
                wb = t % NWB
                for (dst, wv, nm_) in ((w1b[wb][:].rearrange("p a b c -> p (a b c)"), w1b_d, "E_w1b%d" % wb),
                                       (w3b[wb][:].rearrange("p a b c -> p (a b c)"), w3b_d, "E_w3b%d" % wb),
                                       (w2b[wb][:].rearrange("p a b -> p (a b)"), w2b_d, "E_w2b%d" % wb)):
                    P.dma("pool", lambda e, dst=dst, wv=wv, t=t: wfetch(e, dst, wv, t), nm_ + "f", reads=["E_widx"], writes=[nm_])

            def load_x(t):
                P.dma("sp", lambda e, t=t: e.dma_start(out=xs_[t % 2][:], in_=Xs_t[t]), "E_xs%d" % (t % 2), writes=["E_xs%d" % (t % 2)])

            def transposes(t):
                xb_ = xs_[t % 2]
                for kc in range(8):
                    bi = 6 + kc % 2
                    pv = ps[bi][:].bitcast(BF16)
                    for j in range(4):
                        P.op("pe", lambda e, pv=pv, j=j, kc=kc: e.transpose(out=pv[:, j * 128:(j + 1) * 128], in_=xb_[:, j, kc * 128:(kc + 1) * 128], identity=identb[:]),
                             reads=["E_xs%d" % (t % 2), "identb"], writes=["psX%d" % (kc % 2)])
                    if kc % 2 == 0:
                        A(lambda e, pv=pv, kc=kc: e.copy(out=xT[t % 2][:, kc, :], in_=pv[:, 0:512]), ["psX%d" % (kc % 2)], ["E_xT%d_%d" % (t % 2, kc)])
                    else:
                        V(lambda e, pv=pv, kc=kc: e.tensor_copy(out=xT[t % 2][:, kc, :], in_=pv[:, 0:512]), ["psX%d" % (kc % 2)], ["E_xT%d_%d" % (t % 2, kc)])

            def hidden(t):
                wb = t % NWB
                xres = ["E_xT%d_%d" % (t % 2, kc) for kc in range(8)]
                for fc in range(4):
                    b1 = ps[(2 * fc) % 4]
                    b3 = ps[(2 * fc + 1) % 4]
                    n1, n3 = "psG%d" % ((2 * fc) % 4), "psG%d" % ((2 * fc + 1) % 4)
                    sl = sil[fc % 2]
                    sn_ = "E_sil%d" % (fc % 2)
                    for kc in range(8):
                        P.op("pe", lambda e, b1=b1, kc=kc, fc=fc: e.matmul(b1[:, :], lhsT=w1b[wb][:, kc, fc, :], rhs=xT[t % 2][:, kc, :], start=(kc == 0), stop=(kc == 7)),
                             reads=(["E_w1b%d" % wb] + xres) if kc == 0 else [], writes=[n1])
                    for kc in range(8):
                        P.op("pe", lambda e, b3=b3, kc=kc, fc=fc: e.matmul(b3[:, :], lhsT=w3b[wb][:, kc, fc, :], rhs=xT[t % 2][:, kc, :], start=(kc == 0), stop=(kc == 7)),
                             reads=(["E_w3b%d" % wb] + xres) if kc == 0 else [], writes=[n3])
                    A(lambda e, b1=b1, sl=sl: e.activation(out=sl[:], in_=b1[:, :], func=AF.Silu), [n1], [sn_])
                    V(lambda e, b3=b3, sl=sl, fc=fc: e.tensor_tensor(out=hidT[:, fc, :], in0=b3[:, :], in1=sl[:], op=ALU.mult), [n3, sn_], ["E_hidT%d" % fc])

            def outproj(t):
                wb = t % NWB
                hres = ["E_hidT%d" % fc for fc in range(4)]
                yb_ = yq[t % 2]
                ybn = "E_yq%d" % (t % 2)
                for j in range(4):
                    for hf in range(2):
                        bi = 4 + hf
                        bk = ps[bi]
                        bn = "psH%d" % hf
                        for fc in range(4):
                            P.op("pe", lambda e, bk=bk, fc=fc, j=j, hf=hf: e.matmul(bk[:, :], lhsT=hidT[:, fc, j * 128:(j + 1) * 128], rhs=w2b[wb][:, fc, hf * 512:(hf + 1) * 512],
                                                                                 start=(fc == 0), stop=(fc == 3)),
                                 reads=(hres + ["E_w2b%d" % wb]) if fc == 0 else [], writes=[bn])
                        if hf == 0:
                            A(lambda e, bk=bk, j=j: e.copy(out=yb_[:, j, 0:512], in_=bk[:, :]), [bn], [ybn + "_%d_0" % j])
                        else:
                            V(lambda e, bk=bk, j=j: e.tensor_copy(out=yb_[:, j, 512:1024], in_=bk[:, :]), [bn], [ybn + "_%d_1" % j])
                P.dma("act", lambda e: e.dma_start(out=Ys_t[t], in_=yb_[:]), "E_yso%d" % (t % 2), reads=[ybn + "_%d_%d" % (j, hf) for j in range(4) for hf in range(2)])

            for t in range(NWB):
                fetch(t)
            load_x(0)
            transposes(0)
            for t in range(NTL):
                if t + 1 < NTL:
                    load_x(t + 1)
                hidden(t)
                if t + 1 < NTL:
                    transposes(t + 1)
                outproj(t)
                if t + NWB < NTL:
                    fetch(t + NWB)
            P.barrier()
            P.emit()

        with ExitStack() as sc:
            Y1 = [sb("E_Y1_%d" % i, [128, 1024], BF16, sc) for i in range(2)]
            Y2 = [sb("E_Y2_%d" % i, [128, 1024], BF16, sc) for i in range(2)]
            x1b = [sb("E_x1b%d" % i, [128, 1024], F32, sc) for i in range(2)]
            ob_ = [sb("E_ob%d" % i, [128, 1024], F32, sc) for i in range(2)]
            junk = sb("E_junk", [128, 1024], BF16, sc)
            fs = sb("E_fs", [128, 4], F32, sc)
            x1_r = x1_d.rearrange("(n p) d -> n p d", p=128)
            out_r = out_d.rearrange("(n p) d -> n p d", p=128)
            for n_ in range(64):
                b = n_ % 2
                y1, y2, xb, ob = Y1[b], Y2[b], x1b[b], ob_[b]
                y1n, y2n, xbn, obn = "E_Y1_%d" % b, "E_Y2_%d" % b, "E_x1b%d" % b, "E_ob%d" % b
                P.dma("pool", lambda e, y1=y1, n_=n_: e.indirect_dma_start(out=y1[:, :], out_offset=None, in_=Ys_d[:, :], in_offset=IOA(ap=S1i[:, n_:n_ + 1], axis=0)), y1n, reads=["E_S1i"], writes=[y1n])
                P.dma("pool", lambda e, y2=y2, n_=n_: e.indirect_dma_start(out=y2[:, :], out_offset=None, in_=Ys_d[:, :], in_offset=IOA(ap=S2i[:, n_:n_ + 1], axis=0)), y2n, reads=["E_S2i"], writes=[y2n])
                P.dma("sp", lambda e, xb=xb, n_=n_: e.dma_start(out=xb[:], in_=x1_r[n_]), xbn, writes=[xbn])
                V(lambda e, y1=y1, xb=xb, n_=n_: e.scalar_tensor_tensor(out=xb[:], in0=y1[:], scalar=cw1[:, n_:n_ + 1], in1=xb[:], op0=ALU.mult, op1=ALU.add),
                  [y1n, xbn, "cw1"], [xbn])
                V(lambda e, y2=y2, xb=xb, n_=n_: e.scalar_tensor_tensor(out=xb[:], in0=y2[:], scalar=cw2[:, n_:n_ + 1], in1=xb[:], op0=ALU.mult, op1=ALU.add),
                  [y2n, xbn, "cw2"], [xbn])
                A(lambda e, xb=xb: e.activation(out=junk[:], in_=xb[:], func=AF.Square, accum_out=fs[:, 0:1]), [xbn], ["E_junk", "E_fs"])
                A(lambda e: e.activation(out=fs[:, 1:2], in_=fs[:, 0:1], func=AF.Sqrt, scale=1.0 / D, bias=EPS), ["E_fs"], ["E_fs"])
                V(lambda e: e.reciprocal(out=fs[:, 2:3], in_=fs[:, 1:2]), ["E_fs"], ["E_fs"])
                V(lambda e, xb=xb, ob=ob: e.scalar_tensor_tensor(out=ob[:], in0=xb[:], scalar=fs[:, 2:3], in1=gfbc[:], op0=ALU.mult, op1=ALU.mult),
                  [xbn, "E_fs", "E_gfbc"], [obn])
                P.dma("act", lambda e, ob=ob, n_=n_: e.dma_start(out=out_r[n_], in_=ob[:]), obn + "o", reads=[obn])
            P.barrier()
            P.emit()

    es.close()
    return nc


def _host_consts():
    c = {}
    c["identb"] = np.eye(128, dtype=np.float32).astype(ml_dtypes.bfloat16)
    qt = np.arange(64)[:, None]; jj = np.arange(32)[None, :]
    em = np.where(jj < qt // 2, 0.0, -1e30).astype(np.float32)
    c["emask"] = np.ascontiguousarray(np.broadcast_to(em.reshape(1, 2048), (128, 2048)))
    c["emaskm"] = np.ascontiguousarray(np.broadcast_to((em + np.float32(NEG)).reshape(1, 2048), (128, 2048)))
    o0 = np.where(jj == qt // 2, 0.0, NEG).astype(np.float32)
    c["own0"] = np.ascontiguousarray(np.broadcast_to(o0.reshape(1, 2048), (128, 2048)))
    c["identf"] = np.eye(128, dtype=np.float32)
    c["triu"] = np.triu(np.ones((128, 128), np.float32), 1).astype(ml_dtypes.bfloat16)
    c["onesb"] = np.ones((128, 128), np.float32).astype(ml_dtypes.bfloat16)
    c["m512"] = np.ascontiguousarray(np.broadcast_to((np.arange(32, dtype=np.float32) * 512.0)[None, None, :], (128, 32, 32)).reshape(128, 1024))
    c["t512"] = np.ascontiguousarray(np.broadcast_to((np.arange(64, dtype=np.float32) * 512.0)[None, None, :], (128, 32, 64)).reshape(128, 2048))
    c["pcol"] = np.arange(128, dtype=np.float32).reshape(128, 1)
    c["identr"] = np.eye(128, dtype=np.float32)[::-1].copy().astype(ml_dtypes.bfloat16)
    ind = (np.arange(S)[None, :] // 256 == np.arange(32)[:, None]).astype(np.float32)
    c["ind"] = ind.astype(ml_dtypes.bfloat16)
    n = np.arange(-512, 640)
    nn = np.maximum(n, 0)
    large = 16 + (np.log(np.maximum(nn, 16).astype(np.float32) / np.float32(16)) / np.float32(math.log(128 / 16)) * np.float32(16)).astype(np.int32)
    bucket = np.where(nn < 16, nn, np.minimum(large, 31))
    ohp = np.zeros((33, n.size), np.float32)
    for i, (ni, b) in enumerate(zip(n, bucket)):
        if ni >= 0:
            ohp[b, i] += 1.0
            ohp[31, i] -= 1.0
        else:
            ohp[32, i] = 1.0
    c["ohp"] = ohp
    return c


def _prep(inputs):
    f = lambda a: np.ascontiguousarray(np.asarray(a, dtype=np.float32))
    shared = {}
    shared["w_in"] = f(inputs["w_in"][0])
    shared["g1"] = f(np.asarray(inputs["ln1_g"][0]).reshape(8, 128).T)
    shared["bgate"] = f(np.asarray(inputs["b_gate"][0]).reshape(16, 128).T)
    shared.update(_host_consts())
    shared["w_up_ssm"] = f(inputs["w_up_ssm"][0]); shared["w_up_attn"] = f(inputs["w_up_attn"][0]); shared["w_out"] = f(inputs["w_out"][0])
    shared["g2bc"] = f(np.broadcast_to(np.asarray(inputs["ln2_g"][0])[None, :], (128, 1024)))
    shared["gfbc"] = f(np.broadcast_to(np.asarray(inputs["ln_f_g"])[None, :], (128, 1024)))
    shared["wr"] = f(np.concatenate([inputs["w_router_group"][0], inputs["w_router_expert"][0]], axis=1))
    shared["bias36"] = f(np.broadcast_to(np.concatenate([inputs["b_router_group"][0], inputs["b_router_expert"][0]])[None, :], (128, 36)))
    shared["w1"] = f(inputs["w1"][0]); shared["w3"] = f(inputs["w3"][0]); shared["w2"] = f(inputs["w2"][0])
    rb = f(inputs["rel_bias"])
    shared["rb33"] = f(np.concatenate([rb.T, np.full((1, 8), NEG, np.float32)], axis=0))
    shared["rb31"] = f(np.broadcast_to(rb[:, 31][None, :], (128, 8)))
    lre = f(inputs["ssm_lambda_re"][0]); lim = f(inputs["ssm_lambda_im"][0]); lst = f(inputs["ssm_log_step"][0])
    bre = f(inputs["ssm_b_re"][0]); bim = f(inputs["ssm_b_im"][0])
    cre = f(inputs["ssm_c_re"][0]); cim = f(inputs["ssm_c_im"][0])
    ml = lambda a: f(a.reshape(16, 2, 64).transpose(1, 2, 0).reshape(128, 16))
    shared["lre_ml"] = ml(lre); shared["lim_ml"] = ml(lim)
    shared["lst_ml"] = ml(np.repeat(lst[:, None], 64, axis=1))
    def fl(a):
        t = a.reshape(16, 2, 64).reshape(16, 128)
        return f(np.broadcast_to(t[None], (128, 16, 128)).reshape(128, 2048))
    shared["lre_fl"] = fl(lre); shared["lim_fl"] = fl(lim)
    shared["lst_fl"] = fl(np.repeat(lst[:, None], 64, axis=1))
    def bfl(b):
        o = np.zeros((128, 16, 128), np.float32)
        for k in range(16):
            for two in range(2):
                g = 2 * k + two
                r0 = 32 * (k % 4) + 16 * two
                o[r0:r0 + 16, k, two * 64:(two + 1) * 64] = b[g].T
        return o.reshape(128, 2048)
    shared["bre_fl"] = bfl(bre); shared["bim_fl"] = bfl(bim)
    def cml(c):
        o = np.zeros((128, 16, 128), np.float32)
        for k in range(16):
            for two in range(2):
                g = 2 * k + two
                c0 = 32 * (k % 4) + 16 * two
                o[two * 64:(two + 1) * 64, k, c0:c0 + 16] = c[g].T
        return o.reshape(128, 2048)
    shared["cre_ml"] = cml(cre); shared["cim_ml"] = cml(cim)
    shared["d_fm"] = f(np.asarray(inputs["ssm_d"][0]).reshape(4, 128).T)
    shared["wglu"] = f(inputs["w_glu"][0])
    shared["bglu"] = f(np.asarray(inputs["b_glu"][0]).reshape(4, 128).T)
    x = np.asarray(inputs["x"], dtype=np.float32)
    maps = []
    for c in range(8):
        m = dict(shared)
        m["x"] = np.ascontiguousarray(x[c])
        maps.append(m)
    return maps


def kernel(**inputs):
    nc = build()
    maps = _prep(inputs)
    res = run_bass_kernel_spmd(nc, maps, core_ids=list(range(8)))
    out = np.stack([np.asarray(r["out"], dtype=np.float32) for r in res.results], axis=0)
    return out
```

```python
import os
import math
import numpy as np
import ml_dtypes
from contextlib import ExitStack
import concourse.bass as bass
import concourse.mybir as mybir
from concourse.bass_utils import run_bass_kernel_spmd

F32 = mybir.dt.float32
BF16 = mybir.dt.bfloat16
I32 = mybir.dt.int32
AF = mybir.ActivationFunctionType
ALU = mybir.AluOpType
AX = mybir.AxisListType

S = 8192
D = 1024
NT = S // 512
EPS = 1e-6
NEG = -30000.0
COMPUTE = ("pe", "act", "dve", "pool")


class Prog:
    def __init__(self, nc, es):
        self.nc = nc
        self.es = es
        self.ops = {e: [] for e in COMPUTE + ("sp",)}
        self.psem = {e: es.enter_context(nc.semaphore("prog_" + e)) for e in COMPUTE}
        self.cnt = {e: 0 for e in COMPUTE}
        self.dsem = {}
        self.dcnt = {}
        self.res = {}
        self.waited = {e: {} for e in self.ops}
        self.free = {True: [], False: []}
        self.dkind = {}

    def _deps(self, eng, reads, writes):
        need = {}

        def add(tok):
            if tok is None:
                return
            sem, val, src = tok
            if src == "pe" and eng == "pe":
                return
            k = id(sem)
            if k not in need or need[k][1] < val:
                need[k] = (sem, val)

        for r in reads:
            st = self.res.get(r)
            if st:
                add(st[0])
        for r in writes:
            st = self.res.get(r)
            if st:
                add(st[0])
                for t in st[1]:
                    add(t)
        out = []
        for k, (sem, val) in need.items():
            if self.waited[eng].get(k, 0) >= val:
                continue
            self.waited[eng][k] = val
            out.append((sem, val))
        return out

    def _commit(self, tok, reads, writes):
        for r in writes:
            self.res[r] = [tok, []]
        for r in reads:
            if r in writes:
                continue
            st = self.res.setdefault(r, [None, []])
            st[1].append(tok)
            if len(st[1]) > 64:
                st[1] = st[1][-64:]

    def op(self, eng, fn, reads=(), writes=()):
        waits = self._deps(eng, reads, writes)
        self.cnt[eng] += 1
        tok = (self.psem[eng], self.cnt[eng], eng)
        self.ops[eng].append((waits, fn, (self.psem[eng], 1)))
        self._commit(tok, reads, writes)

    def dma(self, eng, fn, key, reads=(), writes=()):
        if key not in self.dsem:
            kind = (eng == "pool")
            self.dkind[key] = kind
            if self.free[kind]:
                self.dsem[key], self.dcnt[key] = self.free[kind].pop()
            else:
                self.dsem[key] = self.es.enter_context(self.nc.semaphore("d_" + key))
                self.dcnt[key] = 0
        assert self.dkind[key] == (eng == "pool"), key
        waits = self._deps(eng, reads, writes)
        self.dcnt[key] += 16
        tok = (self.dsem[key], self.dcnt[key], "dma")
        self.ops[eng].append((waits, fn, (self.dsem[key], 16)))
        self._commit(tok, reads, writes)

    def barrier(self):
        for e in self.ops:
            waits = []
            for c in COMPUTE:
                if self.cnt[c] > 0 and self.waited[e].get(id(self.psem[c]), 0) < self.cnt[c]:
                    self.waited[e][id(self.psem[c])] = self.cnt[c]
                    waits.append((self.psem[c], self.cnt[c]))
            for k, sem in self.dsem.items():
                if self.waited[e].get(id(sem), 0) < self.dcnt[k]:
                    self.waited[e][id(sem)] = self.dcnt[k]
                    waits.append((sem, self.dcnt[k]))
            if waits:
                self.ops[e].append((waits, None, None))
        self.res = {}
        for k in list(self.dsem):
            self.free[self.dkind.pop(k)].append((self.dsem.pop(k), self.dcnt.pop(k)))

    def emit(self):
        nc = self.nc
        ops = self.ops

        def mk(name):
            def f(eng):
                for waits, fn, inc in ops[name]:
                    for sem, val in waits:
                        eng.wait_ge(sem, val)
                    if fn is not None:
                        ins = fn(eng)
                        ins.then_inc(inc[0], inc[1])
            return f

        with nc.Block() as blk:
            blk.sync(mk("sp"))
            blk.scalar(mk("act"))
            blk.vector(mk("dve"))
            blk.gpsimd(mk("pool"))
            blk.tensor(mk("pe"))
        self.ops = {e: [] for e in self.ops}


def build(dbg=None):
    nc = bass.Bass("TRN2", target_bir_lowering=False)
    es = ExitStack()
    P = Prog(nc, es)

    def din(name, shape, dt=F32):
        return nc.dram_tensor(name, list(shape), dt, kind="ExternalInput").ap()

    def dscr(name, shape, dt):
        kind = "ExternalOutput" if (dbg and name in dbg) else "Internal"
        return nc.dram_tensor(name, list(shape), dt, kind=kind).ap()

    def sb(name, shape, dt, st=None):
        return (st or es).enter_context(nc.sbuf_tensor(name, list(shape), dt))

    x_d = din("x", [S, D])
    win_d = din("w_in", [D, 4096])
    g1_d = din("g1", [128, 8])
    bg_d = din("bgate", [128, 16])
    identb_d = din("identb", [128, 128], BF16)
    out_d = nc.dram_tensor("out", [S, D], F32, kind="ExternalOutput").ap()

    uT_d = dscr("uT", [4, 128, S], BF16)
    qT_d = dscr("qT", [4, 128, S], BF16)
    kT_d = dscr("kT", [4, 128, S], BF16)
    v_d = dscr("v", [S, 512], BF16)
    gate_d = dscr("gate", [16, 128, S], BF16)

    ps = [es.enter_context(nc.psum_tensor("ps%d" % i, [128, 512], F32)) for i in range(8)]
    identb = sb("identb_s", [128, 128], BF16)
    P.dma("sp", lambda e: e.dma_start(out=identb[:], in_=identb_d), "ident", writes=["identb"])
    identr_d = din("identr", [128, 128], BF16)
    identr = sb("identr_s", [128, 128], BF16)
    P.dma("sp", lambda e: e.dma_start(out=identr[:], in_=identr_d), "identr", writes=["identr"])

    with ExitStack() as st:
        winb = sb("winb", [128, 8, 4096], BF16, st)
        wstage = [sb("wstage%d" % i, [128, 4096], F32, st) for i in range(2)]
        g1 = sb("g1s", [128, 8], F32, st)
        bg = sb("bgs", [128, 16], F32, st)
        xs = [sb("xs%d" % i, [128, 4, 1024], F32, st) for i in range(2)]
        junk = sb("junk", [128, 1024], BF16, st)
        ss = sb("ss", [128, 4], F32, st)
        rstd = sb("rstd", [128, 4], F32, st)
        xnb = sb("xnb", [128, 4, 1024], BF16, st)
        hT = sb("hT", [128, 8, 512], BF16, st)
        NST = 8
        stg = [sb("stg%d" % i, [128, 512], BF16, st) for i in range(NST)]

        P.dma("sp", lambda e: e.dma_start(out=g1[:], in_=g1_d), "g1", writes=["g1"])
        P.dma("sp", lambda e: e.dma_start(out=bg[:], in_=bg_d), "bg", writes=["bg"])
        for kc in range(8):
            w = wstage[kc % 2]
            P.dma("sp", lambda e, w=w, kc=kc: e.dma_start(out=w[:], in_=win_d[kc * 128:(kc + 1) * 128, :]),
                  "wst%d" % (kc % 2), writes=["wstage%d" % (kc % 2)])
            P.op("dve", lambda e, w=w, kc=kc: e.tensor_scalar(out=winb[:, kc, :], in0=w[:], scalar1=g1[:, kc:kc + 1],
                                                             scalar2=None, op0=ALU.mult),
                 reads=["wstage%d" % (kc % 2), "g1"], writes=["winb"])

        x_t = x_d.rearrange("(t j p) d -> t p j d", j=4, p=128)

        def load_x(T):
            P.dma("sp", lambda e, T=T: e.dma_start(out=xs[T % 2][:], in_=x_t[T]), "xs%d" % (T % 2),
                  writes=["xs%d" % (T % 2)])

        load_x(0)
        nst = 0
        evac = 0
        for T in range(NT):
            if T + 1 < NT:
                load_x(T + 1)
            xt = xs[T % 2]
            xr = "xs%d" % (T % 2)
            for j in range(4):
                P.op("act", lambda e, xt=xt, j=j: e.activation(out=junk[:], in_=xt[:, j, :], func=AF.Square,
                                                               accum_out=ss[:, j:j + 1]),
                     reads=[xr], writes=["junk", "ss"])
            P.op("act", lambda e: e.activation(out=rstd[:], in_=ss[:], func=AF.Sqrt, scale=1.0 / D, bias=EPS),
                 reads=["ss"], writes=["rstd"])
            P.op("dve", lambda e: e.reciprocal(out=rstd[:], in_=rstd[:]), reads=["rstd"], writes=["rstd"])
            for j in range(4):
                P.op("dve", lambda e, xt=xt, j=j: e.tensor_scalar(out=xnb[:, j, :], in0=xt[:, j, :],
                                                                 scalar1=rstd[:, j:j + 1], scalar2=None, op0=ALU.mult),
                     reads=[xr, "rstd"], writes=["xnb%d" % j])
            for kc in range(8):
                bank = ps[kc % 2]
                pv = bank[:].bitcast(BF16)
                for j in range(4):
                    P.op("pe", lambda e, pv=pv, j=j, kc=kc: e.transpose(out=pv[:, j * 128:(j + 1) * 128],
                                                                        in_=xnb[:, j, kc * 128:(kc + 1) * 128],
                                                                        identity=identb[:]),
                         reads=["xnb%d" % j, "identb"], writes=["psA%d" % (kc % 2)])
                eng = "act" if kc % 2 == 0 else "dve"
                if eng == "act":
                    P.op("act", lambda e, pv=pv, kc=kc: e.copy(out=hT[:, kc, :], in_=pv[:, 0:512]),
                         reads=["psA%d" % (kc % 2)], writes=["hT%d" % kc])
                else:
                    P.op("dve", lambda e, pv=pv, kc=kc: e.tensor_copy(out=hT[:, kc, :], in_=pv[:, 0:512]),
                         reads=["psA%d" % (kc % 2)], writes=["hT%d" % kc])
            hres = ["hT%d" % kc for kc in range(8)]
            for fc in list(range(0, 12)) + list(range(16, 32)):
                bank = ps[2 + (evac % 4)]
                br = "psB%d" % (evac % 4)
                for kc in range(8):
                    P.op("pe", lambda e, bank=bank, kc=kc, fc=fc: e.matmul(bank[:], lhsT=winb[:, kc, fc * 128:(fc + 1) * 128],
                                                                            rhs=hT[:, kc, :], start=(kc == 0), stop=(kc == 7)),
                         reads=hres + ["winb"] if kc == 0 else [], writes=[br])
                so = stg[nst % NST]
                sr = "stg%d" % (nst % NST)
                if fc < 4:
                    dst = uT_d[fc, :, T * 512:(T + 1) * 512]
                elif fc < 8:
                    dst = qT_d[fc - 4, :, T * 512:(T + 1) * 512]
                elif fc < 12:
                    dst = kT_d[fc - 8, :, T * 512:(T + 1) * 512]
                else:
                    dst = gate_d[fc - 16, :, T * 512:(T + 1) * 512]
                if fc >= 16:
                    P.op("act", lambda e, bank=bank, so=so, fc=fc: e.activation(out=so[:], in_=bank[:], func=AF.Sigmoid,
                                                                                bias=bg[:, fc - 16:fc - 15], scale=1.0),
                         reads=[br, "bg"], writes=[sr])
                elif 4 <= fc < 8:
                    P.op("dve", lambda e, bank=bank, so=so: e.tensor_scalar(out=so[:], in0=bank[:], scalar1=0.125,
                                                                            scalar2=None, op0=ALU.mult),
                         reads=[br], writes=[sr])
                else:
                    P.op("dve", lambda e, bank=bank, so=so: e.tensor_copy(out=so[:], in_=bank[:]),
                         reads=[br], writes=[sr])
                P.dma("pool", lambda e, so=so, dst=dst: e.dma_start(out=dst, in_=so[:]), sr + "o", reads=[sr])
                nst += 1
                evac += 1
            for j in range(4):
                bank = ps[2 + (evac % 4)]
                br = "psB%d" % (evac % 4)
                for kc in range(8):
                    P.op("pe", lambda e, bank=bank, kc=kc, j=j: e.matmul(bank[:], lhsT=hT[:, kc, j * 128:(j + 1) * 128],
                                                                          rhs=winb[:, kc, 1536:2048], start=(kc == 0), stop=(kc == 7)),
                         reads=hres + ["winb"] if kc == 0 else [], writes=[br])
                so = stg[nst % NST]
                sr = "stg%d" % (nst % NST)
                P.op("act", lambda e, bank=bank, so=so: e.copy(out=so[:], in_=bank[:]), reads=[br], writes=[sr])
                r0 = T * 512 + j * 128
                P.dma("pool", lambda e, so=so, r0=r0: e.dma_start(out=v_d[r0:r0 + 128, :], in_=so[:]), sr + "o", reads=[sr])
                nst += 1
                evac += 1
        P.barrier()
        P.emit()

    if dbg and "stopA" in dbg:
        es.close()
        return nc

    y2T_d = dscr("y2T", [4, 128, S], BF16)
    w1_d = din("w1", [32, 1024, 512]); w3_d = din("w3", [32, 1024, 512]); w2_d = din("w2", [32, 512, 1024])
    Xs_d = dscr("Xs", [64 * 512, D], BF16)
    w1b_d = dscr("w1b", [4096, 4096], BF16); w3b_d = dscr("w3b", [4096, 4096], BF16); w2b_d = dscr("w2b", [4096, 4096], BF16)
    w1v = w1_d.rearrange("e (p kc) n -> (e p) (kc n)", kc=8)
    w3v = w3_d.rearrange("e (p kc) n -> (e p) (kc n)", kc=8)
    w2v = w2_d.rearrange("e (p fc) n -> (e p) (fc n)", fc=4)
    PRM = {}
    for nm, shp in (("lre_ml", [128, 16]), ("lim_ml", [128, 16]), ("lst_ml", [128, 16]),
                    ("lre_fl", [128, 2048]), ("lim_fl", [128, 2048]), ("lst_fl", [128, 2048]),
                    ("bre_fl", [128, 2048]), ("bim_fl", [128, 2048]),
                    ("cre_ml", [128, 2048]), ("cim_ml", [128, 2048]),
                    ("d_fm", [128, 4]), ("wglu", [512, 512]), ("bglu", [128, 4])):
        PRM[nm] = din(nm, shp)
    with ExitStack() as st:
        CH = 256
        NCH = S // CH

        def V(fn, r, w):
            P.op("dve", fn, reads=r, writes=w)

        def A(fn, r, w):
            P.op("act", fn, reads=r, writes=w)

        def G(fn, r, w):
            P.op("pool", fn, reads=r, writes=w)

        def load(nm, shape, src=None, dt=F32, stk=None):
            t = sb("B_" + nm, shape, dt, stk or st)
            P.dma("sp", lambda e: e.dma_start(out=t[:], in_=(src if src is not None else PRM[nm])), "B_" + nm,
                  writes=["B_" + nm])
            return t

        def rot_params(tag, lre, lim, lst, n, stk=None):
            mk = lambda s_: sb("B_%s_%s" % (tag, s_), [128, n], F32, stk or st)
            nm = lambda s_: "B_%s_%s" % (tag, s_)
            step, r, th, c, s, t1, t2 = [mk(x) for x in ("step", "r", "th", "c", "s", "t1", "t2")]
            lren, limn, lstn = ["B_" + x for x in (lre[1], lim[1], lst[1])]
            lre, lim, lst = lre[0], lim[0], lst[0]
            A(lambda e: e.activation(out=step[:], in_=lst[:], func=AF.Exp), [lstn], [nm("step")])
            V(lambda e: e.tensor_tensor(out=th[:], in0=lim[:], in1=step[:], op=ALU.mult), [limn, nm("step")], [nm("th")])
            V(lambda e: e.tensor_tensor(out=t1[:], in0=lre[:], in1=step[:], op=ALU.mult), [lren, nm("step")], [nm("t1")])
            A(lambda e: e.activation(out=r[:], in_=t1[:], func=AF.Exp), [nm("t1")], [nm("r")])
            A(lambda e: e.activation(out=s[:], in_=th[:], func=AF.Sin, scale=1.0 / 16), [nm("th")], [nm("s")])
            V(lambda e: e.tensor_scalar(out=t2[:], in0=th[:], scalar1=1.0 / 16, scalar2=math.pi / 2, op0=ALU.mult, op1=ALU.add),
              [nm("th")], [nm("t2")])
            A(lambda e: e.activation(out=c[:], in_=t2[:], func=AF.Sin), [nm("t2")], [nm("c")])
            for _ in range(4):
                V(lambda e: e.tensor_tensor(out=t1[:], in0=c[:], in1=c[:], op=ALU.mult), [nm("c")], [nm("t1")])
                V(lambda e: e.tensor_tensor(out=t2[:], in0=s[:], in1=s[:], op=ALU.mult), [nm("s")], [nm("t2")])
                V(lambda e: e.scalar_tensor_tensor(out=s[:], in0=s[:], scalar=2.0, in1=c[:], op0=ALU.mult, op1=ALU.mult),
                  [nm("s"), nm("c")], [nm("s")])
                V(lambda e: e.tensor_tensor(out=c[:], in0=t1[:], in1=t2[:], op=ALU.subtract), [nm("t1"), nm("t2")], [nm("c")])
            return dict(r=r, c=c, s=s, t1=t1, t2=t2, rn=nm("r"), cn=nm("c"), sn=nm("s"), t1n=nm("t1"), t2n=nm("t2"), th=th, thn=nm("th"), step=step, stepn=nm("step"))

        lre_ml = load("lre_ml", [128, 16]); lim_ml = load("lim_ml", [128, 16]); lst_ml = load("lst_ml", [128, 16])
        ml = rot_params("ml", (lre_ml, "lre_ml"), (lim_ml, "lim_ml"), (lst_ml, "lst_ml"), 16)
        Ec = sb("B_Ec", [128, 16, CH], F32, st)
        Es = sb("B_Es", [128, 16, CH], F32, st)
        pc = sb("B_pc", [128, 16], F32, st)
        psn = sb("B_psn", [128, 16], F32, st)
        bbre = sb("B_bbre", [128, 16, 128], BF16, st)
        bbim = sb("B_bbim", [128, 16, 128], BF16, st)
        cmre = sb("B_cmre", [128, 16, 128], BF16, st)
        cmim = sb("B_cmim", [128, 16, 128], BF16, st)
        cmren = sb("B_cmren", [128, 16, 128], BF16, st)
        dfm = load("d_fm", [128, 4])
        bglu = load("bglu", [128, 4])
        wglu = sb("B_wglu", [128, 4, 512], BF16, st)
        stp = ExitStack()
        tq1 = sb("B_tq1", [128, 16, CH // 2], F32, stp)
        tq2 = sb("B_tq2", [128, 16, CH // 2], F32, stp)
        V(lambda e: e.memset(Ec[:, :, 0:1], 1.0), [], ["B_Ec"])
        V(lambda e: e.memset(Es[:, :, 0:1], 0.0), [], ["B_Es"])
        V(lambda e: e.tensor_copy(out=pc[:], in_=ml["c"][:]), [ml["cn"]], ["B_pc"])
        V(lambda e: e.tensor_copy(out=psn[:], in_=ml["s"][:]), [ml["sn"]], ["B_psn"])
        n = 1
        while n < CH:
            bcC = pc[:, :].unsqueeze(2).to_broadcast([128, 16, n])
            bcS = psn[:, :].unsqueeze(2).to_broadcast([128, 16, n])
            a1 = tq1[:, :, 0:n]
            a2 = tq2[:, :, 0:n]
            V(lambda e, bcC=bcC, a1=a1, n=n: e.tensor_tensor(out=a1, in0=Ec[:, :, 0:n], in1=bcC, op=ALU.mult), ["B_Ec", "B_pc"], ["B_tq1"])
            V(lambda e, bcS=bcS, a2=a2, n=n: e.tensor_tensor(out=a2, in0=Es[:, :, 0:n], in1=bcS, op=ALU.mult), ["B_Es", "B_psn"], ["B_tq2"])
            V(lambda e, a1=a1, a2=a2, n=n: e.tensor_tensor(out=Ec[:, :, n:2 * n], in0=a1, in1=a2, op=ALU.subtract), ["B_tq1", "B_tq2"], ["B_Ec"])
            V(lambda e, bcS=bcS, a1=a1, n=n: e.tensor_tensor(out=a1, in0=Ec[:, :, 0:n], in1=bcS, op=ALU.mult), ["B_Ec", "B_psn"], ["B_tq1"])
            V(lambda e, bcC=bcC, a2=a2, n=n: e.tensor_tensor(out=a2, in0=Es[:, :, 0:n], in1=bcC, op=ALU.mult), ["B_Es", "B_pc"], ["B_tq2"])
            V(lambda e, a1=a1, a2=a2, n=n: e.tensor_tensor(out=Es[:, :, n:2 * n], in0=a1, in1=a2, op=ALU.add), ["B_tq1", "B_tq2"], ["B_Es"])
            mt1, mt2 = ml["t1"], ml["t2"]
            V(lambda e, mt1=mt1: e.tensor_tensor(out=mt1[:], in0=pc[:], in1=pc[:], op=ALU.mult), ["B_pc"], [ml["t1n"]])
            V(lambda e, mt2=mt2: e.tensor_tensor(out=mt2[:], in0=psn[:], in1=psn[:], op=ALU.mult), ["B_psn"], [ml["t2n"]])
            V(lambda e: e.scalar_tensor_tensor(out=psn[:], in0=psn[:], scalar=2.0, in1=pc[:], op0=ALU.mult, op1=ALU.mult), ["B_psn", "B_pc"], ["B_psn"])
            V(lambda e, mt1=mt1, mt2=mt2: e.tensor_tensor(out=pc[:], in0=mt1[:], in1=mt2[:], op=ALU.subtract), [ml["t1n"], ml["t2n"]], ["B_pc"])
            n *= 2
        lre_fl = load("lre_fl", [128, 2048], stk=stp); lim_fl = load("lim_fl", [128, 2048], stk=stp); lst_fl = load("lst_fl", [128, 2048], stk=stp)
        fl = rot_params("fl", (lre_fl, "lre_fl"), (lim_fl, "lim_fl"), (lst_fl, "lst_fl"), 2048, stk=stp)
        bre = load("bre_fl", [128, 2048], stk=stp); bim = load("bim_fl", [128, 2048], stk=stp)
        nr = fl["step"]; nrn = fl["stepn"]
        den = fl["th"]; denn = fl["thn"]
        t1, t2, c_, s_, r_ = fl["t1"], fl["t2"], fl["c"], fl["s"], fl["r"]
        t1n, t2n, cn, sn, rn = fl["t1n"], fl["t2n"], fl["cn"], fl["sn"], fl["rn"]
        V(lambda e: e.tensor_tensor(out=c_[:], in0=c_[:], in1=r_[:], op=ALU.mult), [cn, rn], [cn])
        V(lambda e: e.tensor_tensor(out=s_[:], in0=s_[:], in1=r_[:], op=ALU.mult), [sn, rn], [sn])
        V(lambda e: e.tensor_scalar(out=nr[:], in0=c_[:], scalar1=-1.0, scalar2=None, op0=ALU.add), [cn], [nrn])
        V(lambda e: e.tensor_tensor(out=t1[:], in0=lre_fl[:], in1=lre_fl[:], op=ALU.mult), ["B_lre_fl"], [t1n])
        V(lambda e: e.tensor_tensor(out=t2[:], in0=lim_fl[:], in1=lim_fl[:], op=ALU.mult), ["B_lim_fl"], [t2n])
        V(lambda e: e.tensor_tensor(out=den[:], in0=t1[:], in1=t2[:], op=ALU.add), [t1n, t2n], [denn])
        V(lambda e: e.reciprocal(out=den[:], in_=den[:]), [denn], [denn])
        V(lambda e: e.tensor_tensor(out=t1[:], in0=nr[:], in1=lre_fl[:], op=ALU.mult), [nrn, "B_lre_fl"], [t1n])
        V(lambda e: e.tensor_tensor(out=t2[:], in0=s_[:], in1=lim_fl[:], op=ALU.mult), [sn, "B_lim_fl"], [t2n])
        V(lambda e: e.tensor_tensor(out=r_[:], in0=t1[:], in1=t2[:], op=ALU.add), [t1n, t2n], [rn])
        V(lambda e: e.tensor_tensor(out=r_[:], in0=r_[:], in1=den[:], op=ALU.mult), [rn, denn], [rn])
        V(lambda e: e.tensor_tensor(out=t1[:], in0=s_[:], in1=lre_fl[:], op=ALU.mult), [sn, "B_lre_fl"], [t1n])
        V(lambda e: e.tensor_tensor(out=t2[:], in0=nr[:], in1=lim_fl[:], op=ALU.mult), [nrn, "B_lim_fl"], [t2n])
        V(lambda e: e.tensor_tensor(out=c_[:], in0=t1[:], in1=t2[:], op=ALU.subtract), [t1n, t2n], [cn])
        V(lambda e: e.tensor_tensor(out=c_[:], in0=c_[:], in1=den[:], op=ALU.mult), [cn, denn], [cn])
        bbre_f = bbre[:].rearrange("p k m -> p (k m)")
        bbim_f = bbim[:].rearrange("p k m -> p (k m)")
        V(lambda e: e.tensor_tensor(out=t1[:], in0=r_[:], in1=bre[:], op=ALU.mult), [rn, "B_bre_fl"], [t1n])
        V(lambda e: e.tensor_tensor(out=t2[:], in0=c_[:], in1=bim[:], op=ALU.mult), [cn, "B_bim_fl"], [t2n])
        V(lambda e: e.tensor_tensor(out=bbre_f, in0=t1[:], in1=t2[:], op=ALU.subtract), [t1n, t2n], ["B_bbre"])
        V(lambda e: e.tensor_tensor(out=t1[:], in0=r_[:], in1=bim[:], op=ALU.mult), [rn, "B_bim_fl"], [t1n])
        V(lambda e: e.tensor_tensor(out=t2[:], in0=c_[:], in1=bre[:], op=ALU.mult), [cn, "B_bre_fl"], [t2n])
        V(lambda e: e.tensor_tensor(out=bbim_f, in0=t1[:], in1=t2[:], op=ALU.add), [t1n, t2n], ["B_bbim"])
        P.dma("sp", lambda e: e.dma_start(out=lre_fl[:], in_=PRM["cre_ml"]), "B_lre_fl", writes=["B_lre_fl"])
        P.dma("sp", lambda e: e.dma_start(out=lim_fl[:], in_=PRM["cim_ml"]), "B_lim_fl", writes=["B_lim_fl"])
        V(lambda e: e.tensor_copy(out=cmre[:].rearrange("p k m -> p (k m)"), in_=lre_fl[:]), ["B_lre_fl"], ["B_cmre"])
        V(lambda e: e.tensor_scalar(out=cmren[:].rearrange("p k m -> p (k m)"), in0=lre_fl[:], scalar1=-1.0, scalar2=None, op0=ALU.mult), ["B_lre_fl"], ["B_cmren"])
        V(lambda e: e.tensor_scalar(out=cmim[:].rearrange("p k m -> p (k m)"), in0=lim_fl[:], scalar1=-1.0, scalar2=None, op0=ALU.mult), ["B_lim_fl"], ["B_cmim"])
        P.dma("sp", lambda e: e.dma_start(out=lst_fl[:].rearrange("p (k n) -> p k n", k=4), in_=PRM["wglu"].rearrange("(k p) n -> p k n", p=128)),
              "B_lst_fl", writes=["B_lst_fl"])
        V(lambda e: e.tensor_copy(out=wglu[:].rearrange("p k n -> p (k n)"), in_=lst_fl[:]), ["B_lst_fl"], ["B_wglu"])

        P.barrier()
        P.emit()
        stp.close()
        ub = [sb("B_u%d" % i, [128, 4, CH], BF16, st) for i in range(2)]
        gre = sb("B_gre", [128, 16, CH], F32, st)
        gim = sb("B_gim", [128, 16, CH], F32, st)
        ini_re = sb("B_inire", [128, 16], F32, st)
        ini_im = sb("B_iniim", [128, 16], F32, st)
        i1 = sb("B_i1", [128, 16], F32, st)
        i2 = sb("B_i2", [128, 16], F32, st)
        V(lambda e: e.memset(ini_re[:], 0.0), [], ["B_inire"])
        V(lambda e: e.memset(ini_im[:], 0.0), [], ["B_iniim"])
        NB = 2
        w1 = [sb("B_w1_%d" % i, [128, CH], F32, st) for i in range(NB)]
        w2 = [sb("B_w2_%d" % i, [128, CH], F32, st) for i in range(NB)]
        gi_re = [sb("B_gire%d" % i, [128, CH], F32, st) for i in range(NB)]
        gi_im = [sb("B_giim%d" % i, [128, CH], F32, st) for i in range(NB)]
        q1 = [sb("B_q1_%d" % i, [128, CH], F32, st) for i in range(NB)]
        q2 = [sb("B_q2_%d" % i, [128, CH], F32, st) for i in range(NB)]
        prd = [[sb("B_prd%d_%d" % (i, c), [128, CH], BF16, st) for c in range(4)] for i in range(4)]
        ysb = [sb("B_y%d" % i, [128, CH], F32, st) for i in range(2)]
        yt = [sb("B_yt%d" % i, [128, CH], F32, st) for i in range(2)]
        ysg = [sb("B_ysg%d" % i, [128, CH], F32, st) for i in range(2)]
        gb = [sb("B_g%d" % i, [128, 4, CH], BF16, st) for i in range(2)]
        zs = [sb("B_z%d" % i, [128, CH], F32, st) for i in range(2)]
        y2 = [sb("B_y2_%d" % i, [128, 4, CH], BF16, st) for i in range(2)]
        uT_v = uT_d.rearrange("k p s -> p k s")
        y2_v = y2T_d.rearrange("k p s -> p k s")

        def load_u(ch):
            P.dma("sp", lambda e, ch=ch: e.dma_start(out=ub[ch % 2][:], in_=uT_v[:, :, ch * CH:(ch + 1) * CH]),
                  "B_u%d" % (ch % 2), writes=["B_u%d" % (ch % 2)])

        SQ = math.sqrt(0.044715)

        def emit_Bu(ch, k):
            u = ub[ch % 2]
            un = "B_u%d" % (ch % 2)
            kq = k // 4
            bank = ps[k % 2]
            bn = "psB%d" % (k % 2)
            P.op("pe", lambda e: e.matmul(bank[:, 0:CH], lhsT=bbre[:, k, :], rhs=u[:, kq, :], start=True, stop=True),
                 reads=[un, "B_bbre"], writes=[bn])
            P.op("pe", lambda e: e.matmul(bank[:, CH:2 * CH], lhsT=bbim[:, k, :], rhs=u[:, kq, :], start=True, stop=True),
                 reads=[un, "B_bbim"], writes=[bn])

        def emit_mid(ch, k, hb):
            b = k % NB
            bank = ps[k % 2]
            bn = "psB%d" % (k % 2)
            Bre = bank[:, 0:CH]
            Bim = bank[:, CH:2 * CH]
            ec = Ec[:, k, :]
            esn = Es[:, k, :]
            n1, n2, ngr, ngi = "B_w1_%d" % b, "B_w2_%d" % b, "B_gire%d" % b, "B_giim%d" % b
            V(lambda e: e.tensor_tensor(out=w1[b][:], in0=Bre, in1=ec, op=ALU.mult), [bn, "B_Ec"], [n1])
            V(lambda e: e.tensor_tensor(out=w2[b][:], in0=Bim, in1=esn, op=ALU.mult), [bn, "B_Es"], [n2])
            V(lambda e: e.tensor_tensor(out=gi_re[b][:], in0=w1[b][:], in1=w2[b][:], op=ALU.add), [n1, n2], [ngr])
            V(lambda e: e.tensor_tensor(out=w1[b][:], in0=Bim, in1=ec, op=ALU.mult), [bn, "B_Ec"], [n1])
            V(lambda e: e.tensor_tensor(out=w2[b][:], in0=Bre, in1=esn, op=ALU.mult), [bn, "B_Es"], [n2])
            V(lambda e: e.tensor_tensor(out=gi_im[b][:], in0=w1[b][:], in1=w2[b][:], op=ALU.subtract), [n1, n2], [ngi])
            rb = ml["r"][:, k:k + 1].to_broadcast([128, CH])
            V(lambda e: e.tensor_tensor_scan(out=gre[:, k, :], data0=rb, data1=gi_re[b][:], initial=ini_re[:, k:k + 1],
                                             op0=ALU.mult, op1=ALU.add), [ngr, ml["rn"], "B_inire"], ["B_gre%d" % k])
            V(lambda e: e.tensor_tensor_scan(out=gim[:, k, :], data0=rb, data1=gi_im[b][:], initial=ini_im[:, k:k + 1],
                                             op0=ALU.mult, op1=ALU.add), [ngi, ml["rn"], "B_iniim"], ["B_gim%d" % k])
            pr = prd[hb]
            G(lambda e: e.tensor_tensor(out=pr[0][:], in0=gre[:, k, :], in1=ec, op=ALU.mult), ["B_gre%d" % k, "B_Ec"], ["B_prd%d_0" % hb])
            G(lambda e: e.tensor_tensor(out=pr[1][:], in0=gim[:, k, :], in1=esn, op=ALU.mult), ["B_gim%d" % k, "B_Es"], ["B_prd%d_1" % hb])
            G(lambda e: e.tensor_tensor(out=pr[2][:], in0=gre[:, k, :], in1=esn, op=ALU.mult), ["B_gre%d" % k, "B_Es"], ["B_prd%d_2" % hb])
            G(lambda e: e.tensor_tensor(out=pr[3][:], in0=gim[:, k, :], in1=ec, op=ALU.mult), ["B_gim%d" % k, "B_Ec"], ["B_prd%d_3" % hb])

        def emit_C(ch, k, hb):
            kq = k // 4
            ybank = ps[2 + kq]
            ycols = slice((ch % 2) * CH, (ch % 2) * CH + CH)
            yn = "psY%d_%d" % (kq, ch % 2)
            first = (k % 4 == 0)
            last = (k % 4 == 3)
            pr = prd[hb]
            P.op("pe", lambda e: e.matmul(ybank[:, ycols], lhsT=cmre[:, k, :], rhs=pr[0][:], start=first, stop=False),
                 reads=["B_prd%d_0" % hb, "B_cmre"], writes=[yn])
            P.op("pe", lambda e: e.matmul(ybank[:, ycols], lhsT=cmren[:, k, :], rhs=pr[1][:], start=False, stop=False),
                 reads=["B_prd%d_1" % hb, "B_cmren"], writes=[yn])
            P.op("pe", lambda e: e.matmul(ybank[:, ycols], lhsT=cmim[:, k, :], rhs=pr[2][:], start=False, stop=False),
                 reads=["B_prd%d_2" % hb, "B_cmim"], writes=[yn])
            P.op("pe", lambda e: e.matmul(ybank[:, ycols], lhsT=cmim[:, k, :], rhs=pr[3][:], start=False, stop=last),
                 reads=["B_prd%d_3" % hb, "B_cmim"], writes=[yn])

        def emit_epi(ch, kq):
            u = ub[ch % 2]
            un = "B_u%d" % (ch % 2)
            ybank = ps[2 + kq]
            ycols = slice((ch % 2) * CH, (ch % 2) * CH + CH)
            yn = "psY%d_%d" % (kq, ch % 2)
            yb = kq % 2
            ynm, ytn, ysn = "B_y%d" % yb, "B_yt%d" % yb, "B_ysg%d" % yb
            gbn = "B_g%d_%d" % (ch % 2, kq)
            V(lambda e: e.scalar_tensor_tensor(out=ysb[yb][:], in0=u[:, kq, :], scalar=dfm[:, kq:kq + 1], in1=ybank[:, ycols], op0=ALU.mult, op1=ALU.add),
              [un, yn, "B_d_fm"], [ynm])
            A(lambda e: e.activation(out=yt[yb][:], in_=ysb[yb][:], func=AF.Square, scale=SQ), [ynm], [ytn])
            A(lambda e: e.activation(out=yt[yb][:], in_=yt[yb][:], func=AF.Identity, bias=1.0, scale=1.0), [ytn], [ytn])
            G(lambda e: e.tensor_tensor(out=yt[yb][:], in0=yt[yb][:], in1=ysb[yb][:], op=ALU.mult), [ytn, ynm], [ytn])
            A(lambda e: e.activation(out=ysg[yb][:], in_=yt[yb][:], func=AF.Sigmoid, scale=1.5957691216057308), [ytn], [ysn])
            G(lambda e: e.tensor_tensor(out=gb[ch % 2][:, kq, :], in0=ysb[yb][:], in1=ysg[yb][:], op=ALU.mult), [ynm, ysn], [gbn])

        def emit_carry():
            V(lambda e: e.tensor_tensor(out=i1[:], in0=gre[:, :, CH - 1], in1=pc[:], op=ALU.mult), ["B_gre%d" % k for k in range(16)] + ["B_pc"], ["B_i1"])
            V(lambda e: e.tensor_tensor(out=i2[:], in0=gim[:, :, CH - 1], in1=psn[:], op=ALU.mult), ["B_gim%d" % k for k in range(16)] + ["B_psn"], ["B_i2"])
            V(lambda e: e.tensor_tensor(out=ini_re[:], in0=i1[:], in1=i2[:], op=ALU.subtract), ["B_i1", "B_i2"], ["B_inire"])
            V(lambda e: e.tensor_tensor(out=i1[:], in0=gre[:, :, CH - 1], in1=psn[:], op=ALU.mult), ["B_gre%d" % k for k in range(16)] + ["B_psn"], ["B_i1"])
            V(lambda e: e.tensor_tensor(out=i2[:], in0=gim[:, :, CH - 1], in1=pc[:], op=ALU.mult), ["B_gim%d" % k for k in range(16)] + ["B_pc"], ["B_i2"])
            V(lambda e: e.tensor_tensor(out=ini_im[:], in0=i1[:], in1=i2[:], op=ALU.add), ["B_i1", "B_i2"], ["B_iniim"])

        def emit_glu(ch):
            gcur = gb[ch % 2]
            gres = ["B_g%d_%d" % (ch % 2, kq) for kq in range(4)]
            for oc in range(4):
                zb = ps[6 + oc % 2]
                zn = "psZ%d" % (oc % 2)
                for kc in range(4):
                    P.op("pe", lambda e, zb=zb, oc=oc, kc=kc: e.matmul(zb[:, 0:CH], lhsT=wglu[:, kc, oc * 128:(oc + 1) * 128], rhs=gcur[:, kc, :],
                                                                      start=(kc == 0), stop=(kc == 3)),
                         reads=(gres + ["B_wglu"]) if kc == 0 else [], writes=[zn])
                A(lambda e, zb=zb, oc=oc: e.activation(out=zs[oc % 2][:], in_=zb[:, 0:CH], func=AF.Sigmoid, bias=bglu[:, oc:oc + 1], scale=1.0),
                  [zn, "B_bglu"], ["B_z%d" % (oc % 2)])
                G(lambda e, oc=oc: e.tensor_tensor(out=y2[ch % 2][:, oc, :], in0=gcur[:, oc, :], in1=zs[oc % 2][:], op=ALU.mult),
                  ["B_z%d" % (oc % 2), "B_g%d_%d" % (ch % 2, oc)], ["B_y2_%d" % (ch % 2)])
            P.dma("act", lambda e: e.dma_start(out=y2_v[:, :, ch * CH:(ch + 1) * CH], in_=y2[ch % 2][:]), "B_y2o%d" % (ch % 2),
                  reads=["B_y2_%d" % (ch % 2)])

        wst = [sb("B_wst%d" % i, [128, 4096], F32, st) for i in range(2)]
        wob = [sb("B_wob%d" % i, [128, 4096], BF16, st) for i in range(2)]
        wunits = [(e_, w_) for e_ in range(32) for w_ in range(3)]

        def w_load(ui):
            e_, w_ = wunits[ui]
            srcv = (w1v, w3v, w2v)[w_]
            P.dma("sp", lambda e: e.dma_start(out=wst[ui % 2][:], in_=srcv[e_ * 128:(e_ + 1) * 128, :]), "B_wst%d" % (ui % 2), writes=["B_wst%d" % (ui % 2)])

        def w_cast(ui):
            e_, w_ = wunits[ui]
            dstv = (w1b_d, w3b_d, w2b_d)[w_]
            si_, so_ = wst[ui % 2], wob[ui % 2]
            if w_ < 2:
                A(lambda e: e.copy(out=so_[:].rearrange("p (kc fc m) -> p kc fc m", kc=8, fc=4), in_=si_[:].rearrange("p (kc m fc) -> p kc fc m", kc=8, fc=4)),
                  ["B_wst%d" % (ui % 2)], ["B_wob%d" % (ui % 2)])
            else:
                A(lambda e: e.copy(out=so_[:], in_=si_[:]), ["B_wst%d" % (ui % 2)], ["B_wob%d" % (ui % 2)])
            P.dma("act", lambda e: e.dma_start(out=dstv[e_ * 128:(e_ + 1) * 128, :], in_=so_[:]), "B_wobo%d" % (ui % 2), reads=["B_wob%d" % (ui % 2)])

        jobs = [(ch, k) for ch in range(NCH) for k in range(16)]
        NJ = len(jobs)
        w_load(0)
        wnext = 0
        load_u(0)
        emit_Bu(0, 0)
        pend_epi = None
        for j, (ch, k) in enumerate(jobs):
            if k == 2 and ch + 1 < NCH:
                load_u(ch + 1)
            if j + 1 < NJ:
                emit_Bu(*jobs[j + 1])
            emit_mid(ch, k, j % 4)
            emit_C(ch, k, j % 4)
            if pend_epi is not None:
                emit_epi(*pend_epi)
                pend_epi = None
            if k % 4 == 3:
                pend_epi = (ch, k // 4)
            if k == 15:
                emit_carry()
            if k == 4 and ch >= 1:
                emit_glu(ch - 1)
            if j % 5 == 2 and wnext < len(wunits):
                if wnext + 1 < len(wunits):
                    w_load(wnext + 1)
                w_cast(wnext)
                wnext += 1
        emit_epi(*pend_epi)
        emit_glu(NCH - 1)
        while wnext < len(wunits):
            if wnext + 1 < len(wunits):
                w_load(wnext + 1)
            w_cast(wnext)
            wnext += 1
        P.barrier()
        P.emit()

    if dbg and "stopB" in dbg:
        es.close()
        return nc

    yaT_d = dscr("yaT", [4, 128, S], BF16)
    LT = 1152
    tab_d = dscr("tab", [8, LT], BF16)
    ind_d = din("ind", [32, S], BF16)
    ohp_d = din("ohp", [33, LT])
    rb33_d = din("rb33", [33, 8])
    rb31_d = din("rb31", [128, 8])
    emask_d = din("emask", [128, 2048])
    emaskm_d = din("emaskm", [128, 2048])
    own0_d = din("own0", [128, 2048])
    with ExitStack() as st:
        def V(fn, r, w):
            P.op("dve", fn, reads=r, writes=w)

        def A(fn, r, w):
            P.op("act", fn, reads=r, writes=w)

        def G(fn, r, w):
            P.op("pool", fn, reads=r, writes=w)

        PSN = lambda i: "PS%d" % i
        QA = [sb("C_QA%d" % i, [96, S], BF16, st) for i in range(2)]
        KA = [sb("C_KA%d" % i, [96, S], BF16, st) for i in range(2)]
        Vh = [sb("C_V%d" % i, [128, 64, 65], BF16, st) for i in range(2)]
        BT = [sb("C_BT%d" % i, [128, 5, 512], BF16, st) for i in range(2)]
        ohp = sb("C_ohp", [33, LT], F32, st)
        rb33 = sb("C_rb33", [33, 8], F32, st)
        ch = sb("C_ch", [128, 8], F32, st)
        tabs = sb("C_tabs", [8, LT], BF16, st)
        kmf = sb("C_kmf", [64, 32], F32, st)
        kmb = sb("C_kmb", [64, 32], BF16, st)
        emask = sb("C_emask", [128, 64, 32], F32, st)
        emaskm = sb("C_emaskm", [128, 64, 32], F32, st)
        own0 = sb("C_own0", [128, 64, 32], F32, st)
        Gt = sb("C_G", [128, 64, 32], F32, st)
        Tt = sb("C_T", [128, 64, 32], F32, st)
        mx = sb("C_mx", [128, 64, 8], F32, st)
        Mall = sb("C_Mall", [128, 64, 128], BF16, st)
        Pt = [sb("C_Pt%d" % i, [128, 512], BF16, st) for i in range(5)]
        rd = sb("C_rd", [65, 512], F32, st)
        onesf = sb("C_onesf", [65, 64], F32, st)
        bcs = sb("C_bcs", [64, 512], F32, st)
        yo = [sb("C_yo%d" % i, [64, 512], BF16, st) for i in range(2)]

        for i in range(2):
            P.dma("sp", lambda e, i=i: e.dma_start(out=KA[i][64:96, :], in_=ind_d), "C_ind%d" % i, writes=["C_KAind%d" % i])
            V(lambda e, i=i: e.memset(Vh[i][:, :, 64:65], 1.0), [], ["C_Vones%d" % i])
        P.dma("sp", lambda e: e.dma_start(out=ohp[:], in_=ohp_d), "C_ohp", writes=["C_ohp"])
        P.dma("sp", lambda e: e.dma_start(out=rb33[:], in_=rb33_d), "C_rb33", writes=["C_rb33"])
        P.dma("sp", lambda e: e.dma_start(out=ch[:], in_=rb31_d), "C_ch", writes=["C_ch"])
        P.dma("sp", lambda e: e.dma_start(out=emask[:].rearrange("p a b -> p (a b)"), in_=emask_d), "C_emask", writes=["C_emask"])
        P.dma("sp", lambda e: e.dma_start(out=emaskm[:].rearrange("p a b -> p (a b)"), in_=emaskm_d), "C_emaskm", writes=["C_emaskm"])
        P.dma("sp", lambda e: e.dma_start(out=own0[:].rearrange("p a b -> p (a b)"), in_=own0_d), "C_own0", writes=["C_own0"])
        V(lambda e: e.memset(onesf[:], 1.0), [], ["C_onesf"])
        G(lambda e: e.memset(Mall[:].rearrange("p a b -> p (a b)"), 0.0), [], ["C_Mall"])
        for c0 in range(0, LT, 384):
            P.op("pe", lambda e, c0=c0: e.matmul(ps[0][0:8, 0:384], lhsT=rb33[:, :], rhs=ohp[:, c0:c0 + 384], start=True, stop=True),
                 reads=["C_rb33", "C_ohp"], writes=[PSN(0)])
            V(lambda e, c0=c0: e.tensor_copy(out=tabs[:, c0:c0 + 384], in_=ps[0][0:8, 0:384]), [PSN(0)], ["C_tabs"])
        P.dma("sp", lambda e: e.dma_start(out=tab_d, in_=tabs[:]), "C_tabo", reads=["C_tabs"], writes=["C_tabd"])

        qT_v = qT_d.rearrange("k (a p) s -> (k a) p s", a=2)
        kT_v = kT_d.rearrange("k (a p) s -> (k a) p s", a=2)
        yaT_v = yaT_d.rearrange("k (a p) s -> (k a) p s", a=2)
        v_v = v_d.rearrange("(t p) (h d) -> h p t d", p=128, d=64)
        cnt = dict(s=0, o=0)

        def loads(h):
            b = h % 2
            P.dma("sp", lambda e: e.dma_start(out=QA[b][0:64, :], in_=qT_v[h]), "C_q%d" % b, writes=["C_QAq%d" % b])
            P.dma("sp", lambda e: e.dma_start(out=KA[b][0:64, :], in_=kT_v[h]), "C_k%d" % b, writes=["C_KAk%d" % b])
            P.dma("sp", lambda e: e.dma_start(out=Vh[b][:, :, 0:64], in_=v_v[h]), "C_v%d" % b, writes=["C_Vd%d" % b])
            for ri in range(5):
                rel = -128 + 128 * ri
                src_ap = bass.AP(tensor=tab_d.tensor, offset=h * LT + 385 - rel, ap=[[1, 128], [1, 512]])
                P.dma("sp", lambda e, ri=ri, src_ap=src_ap: e.dma_start(out=BT[b][:, ri, :], in_=src_ap), "C_bt%d" % b, reads=["C_tabd"], writes=["C_BT%d" % b])

        def gate_units(h):
            b = h % 2
            units = []

            def u_kmean():
                V(lambda e: e.tensor_reduce(out=kmf[:], in_=KA[b][0:64, :].rearrange("p (j n) -> p j n", n=256), op=ALU.add, axis=AX.X),
                  ["C_KAk%d" % b], ["C_kmf"])
                V(lambda e: e.tensor_scalar(out=kmb[:], in0=kmf[:], scalar1=1.0 / 256, scalar2=None, op0=ALU.mult), ["C_kmf"], ["C_kmb"])
            units.append(u_kmean)

            def mk_g1(g4):
                def u():
                    for q16 in range(16):
                        qt = g4 * 16 + q16
                        P.op("pe", lambda e, qt=qt, q16=q16: e.matmul(ps[7][:, q16 * 32:q16 * 32 + 32], lhsT=QA[b][0:64, qt * 128:(qt + 1) * 128], rhs=kmb[:, :],
                                                                     start=True, stop=True),
                             reads=["C_QAq%d" % b, "C_kmb"], writes=[PSN(7)])
                    V(lambda e: e.tensor_tensor(out=Gt[:, g4 * 16:(g4 + 1) * 16, :].rearrange("p a b -> p (a b)"), in0=ps[7][:, :],
                                                in1=emask[:, g4 * 16:(g4 + 1) * 16, :].rearrange("p a b -> p (a b)"), op=ALU.add),
                      [PSN(7), "C_emask"], ["C_G"])
                return u
            for g4 in range(4):
                units.append(mk_g1(g4))

            def u_sel():
                for qt in range(64):
                    V(lambda e, qt=qt: e.max(out=mx[:, qt, :], in_=Gt[:, qt, :]), ["C_G"], ["C_mx%d" % qt])
                V(lambda e: e.tensor_tensor(out=Tt[:], in0=Gt[:], in1=mx[:, :, 2:3].to_broadcast([128, 64, 32]), op=ALU.is_ge),
                  ["C_G"] + ["C_mx%d" % qt for qt in range(64)], ["C_T"])
                V(lambda e: e.scalar_tensor_tensor(out=Tt[:].rearrange("p a b -> p (a b)"), in0=Tt[:].rearrange("p a b -> p (a b)"), scalar=-NEG,
                                                   in1=emaskm[:].rearrange("p a b -> p (a b)"), op0=ALU.mult, op1=ALU.add), ["C_T", "C_emaskm"], ["C_T"])
                V(lambda e: e.tensor_tensor(out=Tt[:].rearrange("p a b -> p (a b)"), in0=Tt[:].rearrange("p a b -> p (a b)"),
                                            in1=own0[:].rearrange("p a b -> p (a b)"), op=ALU.max), ["C_T", "C_own0"], ["C_T"])
                V(lambda e: e.tensor_scalar(out=Mall[:, :, 64:96], in0=Tt[:], scalar1=ch[:, h:h + 1], scalar2=None, op0=ALU.add), ["C_T", "C_ch"], ["C_Mall"])
            units.append(u_sel)

            def mk_g2(g):
                def u():
                    for i4 in range(4):
                        qt = g * 4 + i4
                        P.op("pe", lambda e, qt=qt, i4=i4: e.matmul(ps[7][:, i4 * 128:(i4 + 1) * 128], lhsT=Mall[:, qt, :], rhs=identb[:, :], start=True, stop=True),
                             reads=["C_Mall", "identb"], writes=[PSN(7)])
                    V(lambda e: e.tensor_copy(out=QA[b][64:96, g * 512:(g + 1) * 512], in_=ps[7][64:96, :]), [PSN(7)], ["C_QAm%d" % b])
                return u
            for g in range(16):
                units.append(mk_g2(g))
            return units

        SB = [0, 1, 2, 3, 4]
        LOOK = 4
        FDELAY = 16

        def main(h, side_units):
            b = h % 2
            qres = ["C_QAq%d" % b, "C_QAm%d" % b]
            kres = ["C_KAk%d" % b, "C_KAind%d" % b]
            jobs = [(sbk, kt, 4 * (sbk + 1)) for sbk in range(16) for kt in range(4 * (sbk + 1))]
            slot = {}
            pend = []

            def emit_S(i):
                sbk, kt, nkt = jobs[i]
                qc = slice(sbk * 512, (sbk + 1) * 512)
                si = cnt["s"] % len(SB)
                cnt["s"] += 1
                slot[i] = si
                sbi = SB[si]
                sbn = ps[sbi]
                pt = Pt[si]
                pn = "C_Pt%d" % si
                rel = kt * 128 - sbk * 512
                near = rel >= -128
                P.op("pe", lambda e: e.matmul(sbn[:, :], lhsT=KA[b][:, kt * 128:(kt + 1) * 128], rhs=QA[b][:, qc], start=True, stop=not near),
                     reads=qres + kres, writes=[PSN(sbi)])
                if near:
                    ri = (rel + 128) // 128
                    P.op("pe", lambda e: e.matmul(sbn[:, :], lhsT=identr[:, :], rhs=BT[b][:, ri, :], start=False, stop=True),
                         reads=["C_BT%d" % b, "identr"], writes=[PSN(sbi)])
                A(lambda e: e.activation(out=pt[:], in_=sbn[:, :], func=AF.Exp), [PSN(sbi)], [pn])

            def emit_fin(sbk):
                qc = slice(sbk * 512, (sbk + 1) * 512)
                obi = 5 + sbk % 2
                ob = ps[obi]
                V(lambda e: e.reciprocal(out=rd[64:65, :], in_=ob[64:65, :]), [PSN(obi)], ["C_rd"])
                P.op("pe", lambda e: e.matmul(ps[7][0:64, :], lhsT=onesf[64:65, :], rhs=rd[64:65, :], start=True, stop=True),
                     reads=["C_rd", "C_onesf"], writes=[PSN(7)])
                V(lambda e: e.tensor_copy(out=bcs[:], in_=ps[7][0:64, :]), [PSN(7)], ["C_bcs"])
                yb = yo[sbk % 2]
                yn = "C_yo%d" % (sbk % 2)
                V(lambda e: e.tensor_tensor(out=yb[:], in0=ob[0:64, :], in1=bcs[:], op=ALU.mult), [PSN(obi), "C_bcs"], [yn])
                P.dma("pool", lambda e: e.dma_start(out=yaT_v[h][:, qc], in_=yb[:]), yn + "o", reads=[yn])

            def emit_PV(i):
                sbk, kt, nkt = jobs[i]
                if kt == 0:
                    while pend and pend[0][1] <= sbk - 2:
                        emit_fin(pend.pop(0)[1])
                si = slot.pop(i)
                pt = Pt[si]
                pn = "C_Pt%d" % si
                obi = 5 + sbk % 2
                ob = ps[obi]
                P.op("pe", lambda e: e.matmul(ob[0:65, :], lhsT=Vh[b][:, kt, :], rhs=pt[:], start=(kt == 0), stop=(kt == nkt - 1)),
                     reads=[pn, "C_Vd%d" % b, "C_Vones%d" % b], writes=[PSN(obi)])
                if kt == nkt - 1:
                    pend.append((i + FDELAY, sbk))

            NJ = len(jobs)
            nu = len(side_units)
            ustep = max(1, (NJ - 40) // (nu + 1)) if nu else 0
            ui = 0
            for i in range(NJ + LOOK):
                if i < NJ:
                    emit_S(i)
                if i >= LOOK:
                    emit_PV(i - LOOK)
                while pend and pend[0][0] <= i:
                    emit_fin(pend.pop(0)[1])
                if nu and ui < nu and i >= 20 and (i - 20) % ustep == 0:
                    side_units[ui]()
                    ui += 1
            while pend:
                emit_fin(pend.pop(0)[1])
            while ui < nu:
                side_units[ui]()
                ui += 1

        zt = sb("C_zt", [128, 4096], BF16, st)
        G(lambda e: e.memset(zt[:], 0.0), [], ["C_zt"])
        loads(0)
        for u in gate_units(0):
            u()
        for h in range(8):
            if h + 1 < 8:
                loads(h + 1)
            for zi in range(8 * h, 8 * h + 8):
                P.dma("sp", lambda e, zi=zi: e.dma_start(out=Xs_d[zi * 512:(zi + 1) * 512, :].rearrange("(p r) d -> p (r d)", r=4), in_=zt[:]), "C_zfill", reads=["C_zt"])
            main(h, gate_units(h + 1) if h + 1 < 8 else [])
        P.barrier()
        P.emit()

    if dbg and "stopC" in dbg:
        es.close()
        return nc

    x1_d = dscr("x1", [S, D], F32)
    wups_d = din("w_up_ssm", [512, 1024]); wupa_d = din("w_up_attn", [512, 1024]); wout_d = din("w_out", [1024, 1024])
    g2bc_d = din("g2bc", [128, 1024]); gfbc_d = din("gfbc", [128, 1024])
    wr_d = din("wr", [1024, 36]); bias36_d = din("bias36", [128, 36])
    identf_d = din("identf", [128, 128])
    M1 = sb("M1", [128, 64, 32], F32)
    M2 = sb("M2", [128, 64, 32], F32)
    cw1 = sb("cw1", [128, 64], F32)
    cw2 = sb("cw2", [128, 64], F32)
    xn2p_d = dscr("xn2p", [S, D], BF16)
    with ExitStack() as st:
        def V(fn, r, w):
            P.op("dve", fn, reads=r, writes=w)

        def A(fn, r, w):
            P.op("act", fn, reads=r, writes=w)

        def G(fn, r, w):
            P.op("pool", fn, reads=r, writes=w)

        wups = sb("D_wups", [128, 4, 1024], BF16, st)
        wupa = sb("D_wupa", [128, 4, 1024], BF16, st)
        woutb = sb("D_wout", [128, 8, 1024], BF16, st)
        stage = sb("D_stage", [128, 4096], F32, st)
        g2bc = sb("D_g2bc", [128, 1024], F32, st)
        wr = sb("D_wr", [128, 8, 36], F32, st)
        bias36 = sb("D_bias36", [128, 36], F32, st)
        identf = sb("D_identf", [128, 128], F32, st)
        P.dma("sp", lambda e: e.dma_start(out=g2bc[:], in_=g2bc_d), "D_g2", writes=["D_g2bc"])
        P.dma("sp", lambda e: e.dma_start(out=wr[:], in_=wr_d.rearrange("(k p) n -> p k n", p=128)), "D_wr", writes=["D_wr"])
        P.dma("sp", lambda e: e.dma_start(out=bias36[:], in_=bias36_d), "D_b36", writes=["D_bias36"])
        P.dma("sp", lambda e: e.dma_start(out=identf[:], in_=identf_d), "D_idf", writes=["D_identf"])
        for (wd, wt, nk, nm) in ((wups_d, wups, 4, "D_wups"), (wupa_d, wupa, 4, "D_wupa"), (wout_d[0:512, :], woutb, 4, "D_wout0"), (wout_d[512:1024, :], woutb, 4, "D_wout1")):
            koff = 4 if nm == "D_wout1" else 0
            P.dma("sp", lambda e, wd=wd: e.dma_start(out=stage[:].rearrange("p (k n) -> p k n", k=4), in_=wd.rearrange("(k p) n -> p k n", p=128)),
                  "D_stage", writes=["D_stage"])
            V(lambda e, wt=wt, koff=koff: e.tensor_copy(out=wt[:, koff:koff + 4, :].rearrange("p k n -> p (k n)"), in_=stage[:]), ["D_stage"], [nm])
        wres = ["D_wups", "D_wupa", "D_wout0", "D_wout1"]

        y2t = [sb("D_y2t%d" % i, [128, 4, 512], BF16, st) for i in range(2)]
        yat = [sb("D_yat%d" % i, [128, 4, 512], BF16, st) for i in range(2)]
        gt = [sb("D_gt%d" % i, [128, 16, 512], BF16, st) for i in range(2)]
        m1 = [sb("D_m1_%d" % i, [128, 512], F32, st) for i in range(2)]
        m2 = [sb("D_m2_%d" % i, [128, 512], F32, st) for i in range(2)]
        mT = sb("D_mT", [128, 8, 512], BF16, st)
        xs = sb("D_xs", [128, 4, 1024], F32, st)
        x1s = sb("D_x1s", [128, 4, 1024], F32, st)
        junk = sb("D_junk", [128, 1024], BF16, st)
        ss = sb("D_ss", [128, 4], F32, st)
        rstd = sb("D_rstd", [128, 4], F32, st)
        xn2f = [sb("D_xn2f%d" % i, [128, 1024], F32, st) for i in range(4)]
        xn2b = sb("D_xn2b", [128, 4, 1024], BF16, st)
        xT32 = [sb("D_xT32_%d" % i, [128, 8, 128], F32, st) for i in range(2)]
        L = sb("D_L", [128, 36], F32, st)
        sm = sb("D_sm", [128, 16], F32, st)
        ohg = sb("D_ohg", [128, 4], F32, st)
        msk = sb("D_msk", [128, 32], F32, st)
        mx = sb("D_mx", [128, 8], F32, st)
        ta = sb("D_ta", [128, 32], F32, st)
        tb_ = sb("D_tb", [128, 32], F32, st)
        y2_v = y2T_d.rearrange("k p s -> p k s")
        ya_v = yaT_d.rearrange("k p s -> p k s")
        gate_v = gate_d.rearrange("k p s -> p k s")
        x_t = x_d.rearrange("(t j p) d -> t p j d", j=4, p=128)
        x1_t = x1_d.rearrange("(t j p) d -> t p j d", j=4, p=128)
        xn2p_t = xn2p_d.rearrange("(t j p) d -> t p j d", j=4, p=128)

        def loads(T):
            cs = slice(T * 512, (T + 1) * 512)
            P.dma("sp", lambda e, T=T, cs=cs: e.dma_start(out=y2t[T % 2][:], in_=y2_v[:, :, cs]), "D_y2t%d" % (T % 2), writes=["D_y2t%d" % (T % 2)])
            P.dma("sp", lambda e, T=T, cs=cs: e.dma_start(out=yat[T % 2][:], in_=ya_v[:, :, cs]), "D_yat%d" % (T % 2), writes=["D_yat%d" % (T % 2)])
            P.dma("sp", lambda e, T=T, cs=cs: e.dma_start(out=gt[T % 2][:], in_=gate_v[:, :, cs]), "D_gt%d" % (T % 2), writes=["D_gt%d" % (T % 2)])

        loads(0)
        ev = 0
        for T in range(NT):
            cs = slice(T * 512, (T + 1) * 512)
            P.dma("sp", lambda e, T=T: e.dma_start(out=xs[:], in_=x_t[T]), "D_xs", writes=["D_xs"])
            if T + 1 < NT:
                loads(T + 1)
            yS, yA = y2t[T % 2], yat[T % 2]
            for fc in range(8):
                bS = ps[(2 * ev) % 4]
                bA = ps[(2 * ev + 1) % 4]
                nS, nA = "psD%d" % ((2 * ev) % 4), "psD%d" % ((2 * ev + 1) % 4)
                for kc in range(4):
                    P.op("pe", lambda e, bS=bS, kc=kc, fc=fc, yS=yS: e.matmul(bS[:, :], lhsT=wups[:, kc, fc * 128:(fc + 1) * 128], rhs=yS[:, kc, :],
                                                                             start=(kc == 0), stop=(kc == 3)),
                         reads=(wres + ["D_y2t%d" % (T % 2)]) if kc == 0 else [], writes=[nS])
                for kc in range(4):
                    P.op("pe", lambda e, bA=bA, kc=kc, fc=fc, yA=yA: e.matmul(bA[:, :], lhsT=wupa[:, kc, fc * 128:(fc + 1) * 128], rhs=yA[:, kc, :],
                                                                             start=(kc == 0), stop=(kc == 3)),
                         reads=(wres + ["D_yat%d" % (T % 2)]) if kc == 0 else [], writes=[nA])
                b = ev % 2
                V(lambda e, bS=bS, fc=fc, b=b, T=T: e.tensor_tensor(out=m1[b][:], in0=bS[:, :], in1=gt[T % 2][:, fc, :], op=ALU.mult), [nS, "D_gt%d" % (T % 2)], ["D_m1_%d" % b])
                V(lambda e, bA=bA, fc=fc, b=b, T=T: e.tensor_tensor(out=m2[b][:], in0=bA[:, :], in1=gt[T % 2][:, 8 + fc, :], op=ALU.mult), [nA, "D_gt%d" % (T % 2)], ["D_m2_%d" % b])
                G(lambda e, fc=fc, b=b: e.tensor_tensor(out=mT[:, fc, :], in0=m1[b][:], in1=m2[b][:], op=ALU.add), ["D_m1_%d" % b, "D_m2_%d" % b], ["D_mT%d" % fc])
                ev += 1
            mres = ["D_mT%d" % fc for fc in range(8)]
            for j in range(4):
                for hf in range(2):
                    bk = ps[4 + hf]
                    bn = "psE%d" % hf
                    for kc in range(8):
                        P.op("pe", lambda e, bk=bk, kc=kc, j=j, hf=hf: e.matmul(bk[:, :], lhsT=mT[:, kc, j * 128:(j + 1) * 128], rhs=woutb[:, kc, hf * 512:(hf + 1) * 512],
                                                                               start=(kc == 0), stop=(kc == 7)),
                             reads=(mres + wres) if kc == 0 else [], writes=[bn])
                    V(lambda e, bk=bk, j=j, hf=hf: e.tensor_tensor(out=x1s[:, j, hf * 512:(hf + 1) * 512], in0=bk[:, :], in1=xs[:, j, hf * 512:(hf + 1) * 512], op=ALU.add),
                      [bn, "D_xs"], ["D_x1s%d" % j])
                A(lambda e, j=j: e.activation(out=junk[:], in_=x1s[:, j, :], func=AF.Square, accum_out=ss[:, j:j + 1]), ["D_x1s%d" % j], ["D_junk", "D_ss%d" % j])
            P.dma("act", lambda e, T=T: e.dma_start(out=x1_t[T], in_=x1s[:]), "D_x1o", reads=["D_x1s%d" % j for j in range(4)])
            A(lambda e: e.activation(out=rstd[:], in_=ss[:], func=AF.Sqrt, scale=1.0 / D, bias=EPS), ["D_ss%d" % j for j in range(4)], ["D_rstd"])
            V(lambda e: e.reciprocal(out=rstd[:], in_=rstd[:]), ["D_rstd"], ["D_rstd"])
            for j in range(4):
                xf = xn2f[j]
                xfn = "D_xn2f%d" % j
                V(lambda e, j=j, xf=xf: e.scalar_tensor_tensor(out=xf[:], in0=x1s[:, j, :], scalar=rstd[:, j:j + 1], in1=g2bc[:], op0=ALU.mult, op1=ALU.mult),
                  ["D_x1s%d" % j, "D_rstd", "D_g2bc"], [xfn])

            def tbanks(j):
                if j % 2 == 0:
                    return (ps[6], "psF0"), (ps[7], "psF1"), (ps[2], "psD2")
                return (ps[0], "psD0"), (ps[1], "psD1"), (ps[3], "psD3")

            def r_trans(j):
                xf = xn2f[j]
                xfn = "D_xn2f%d" % j
                (ba, bna), (bb, bnb), _ = tbanks(j)
                x32 = xT32[j % 2]
                for kc in range(8):
                    bk, bn_ = (ba, bna) if kc < 4 else (bb, bnb)
                    P.op("pe", lambda e, kc=kc, bk=bk: e.transpose(out=bk[:, (kc % 4) * 128:(kc % 4 + 1) * 128], in_=xf[:, kc * 128:(kc + 1) * 128], identity=identf[:]),
                         reads=[xfn, "D_identf"], writes=[bn_])
                A(lambda e: e.copy(out=x32[:, 0:4, :].rearrange("p k n -> p (k n)"), in_=ba[:, :]), [bna], ["D_xT32a%d" % (j % 2)])
                A(lambda e: e.copy(out=x32[:, 4:8, :].rearrange("p k n -> p (k n)"), in_=bb[:, :]), [bnb], ["D_xT32b%d" % (j % 2)])

            def r_logits(j):
                _, _, (br, brn) = tbanks(j)
                x32 = xT32[j % 2]
                for kc in range(8):
                    P.op("pe", lambda e, kc=kc: e.matmul(br[:, 0:36], lhsT=x32[:, kc, :], rhs=wr[:, kc, :], start=(kc == 0), stop=(kc == 7)),
                         reads=["D_xT32a%d" % (j % 2), "D_xT32b%d" % (j % 2), "D_wr"] if kc == 0 else [], writes=[brn])

            r_trans(0)
            r_trans(1)
            for j in range(4):
                r_logits(j)
                if j + 2 < 4:
                    r_trans(j + 2)
                _, _, (br, brn) = tbanks(j)
                idx = T * 4 + j
                V(lambda e, br=br: e.tensor_tensor(out=L[:], in0=br[:, 0:36], in1=bias36[:], op=ALU.add), [brn, "D_bias36"], ["D_L"])
                V(lambda e: e.tensor_reduce(out=sm[:, 0:1], in_=L[:, 0:4], op=ALU.max, axis=AX.X), ["D_L"], ["D_sm"])
                V(lambda e: e.tensor_scalar(out=ohg[:], in0=L[:, 0:4], scalar1=sm[:, 0:1], scalar2=None, op0=ALU.is_ge), ["D_L", "D_sm"], ["D_ohg"])
                V(lambda e: e.tensor_scalar(out=sm[:, 1:2], in0=sm[:, 0:1], scalar1=-1.0, scalar2=None, op0=ALU.mult), ["D_sm"], ["D_sm"])
                A(lambda e: e.activation(out=ta[:, 0:4], in_=L[:, 0:4], func=AF.Exp, bias=sm[:, 1:2], scale=1.0, accum_out=sm[:, 2:3]), ["D_L", "D_sm"], ["D_ta", "D_sm"])
                V(lambda e: e.reciprocal(out=sm[:, 3:4], in_=sm[:, 2:3]), ["D_sm"], ["D_sm"])
                V(lambda e: e.tensor_scalar(out=ohg[:], in0=ohg[:], scalar1=1e30, scalar2=-1e30, op0=ALU.mult, op1=ALU.add), ["D_ohg"], ["D_ohg"])
                V(lambda e: e.tensor_tensor(out=msk[:].rearrange("p (g n) -> p g n", g=4), in0=L[:, 4:36].rearrange("p (g n) -> p g n", g=4),
                                            in1=ohg[:, :].unsqueeze(2).to_broadcast([128, 4, 8]), op=ALU.add), ["D_L", "D_ohg"], ["D_msk"])
                V(lambda e: e.max(out=mx[:], in_=msk[:]), ["D_msk"], ["D_mx"])
                V(lambda e: e.tensor_tensor(out=sm[:, 4:5], in0=mx[:, 1:2], in1=mx[:, 0:1], op=ALU.subtract), ["D_mx"], ["D_sm"])
                A(lambda e: e.activation(out=sm[:, 5:6], in_=sm[:, 4:5], func=AF.Exp), ["D_sm"], ["D_sm"])
                V(lambda e: e.tensor_scalar(out=sm[:, 6:7], in0=sm[:, 5:6], scalar1=1.0, scalar2=None, op0=ALU.add), ["D_sm"], ["D_sm"])
                V(lambda e: e.reciprocal(out=sm[:, 6:7], in_=sm[:, 6:7]), ["D_sm"], ["D_sm"])
                V(lambda e: e.tensor_tensor(out=sm[:, 7:8], in0=sm[:, 5:6], in1=sm[:, 6:7], op=ALU.mult), ["D_sm"], ["D_sm"])
                V(lambda e, idx=idx: e.tensor_tensor(out=cw1[:, idx:idx + 1], in0=sm[:, 6:7], in1=sm[:, 3:4], op=ALU.mult), ["D_sm"], ["cw1"])
                V(lambda e, idx=idx: e.tensor_tensor(out=cw2[:, idx:idx + 1], in0=sm[:, 7:8], in1=sm[:, 3:4], op=ALU.mult), ["D_sm"], ["cw2"])
                V(lambda e, idx=idx: e.tensor_scalar(out=M1[:, idx, :], in0=msk[:], scalar1=mx[:, 0:1], scalar2=None, op0=ALU.is_equal), ["D_msk", "D_mx"], ["M1"])
                V(lambda e, idx=idx: e.tensor_scalar(out=M2[:, idx, :], in0=msk[:], scalar1=mx[:, 1:2], scalar2=None, op0=ALU.is_equal), ["D_msk", "D_mx"], ["M2"])
            for j in range(4):
                A(lambda e, j=j: e.copy(out=xn2b[:, j, :].rearrange("q (kc p) -> q kc p", kc=8), in_=xn2f[j][:].rearrange("q (p kc) -> q kc p", kc=8)),
                  ["D_xn2f%d" % j], ["D_xn2b%d" % j])
            P.dma("pool", lambda e, T=T: e.dma_start(out=xn2p_t[T], in_=xn2b[:]), "D_xnTo", reads=["D_xn2b%d" % j for j in range(4)])
        P.barrier()
        P.emit()

    if dbg and "stopD" in dbg:
        es.close()
        return nc

    NTL = 64
    NSL = NTL * 512
    Ys_d = dscr("Ys", [NSL, D], BF16)
    triu_d = din("triu", [128, 128], BF16)
    onesb_d = din("onesb", [128, 128], BF16)
    m512_d = din("m512", [128, 1024])
    t512_d = din("t512", [128, 2048])
    pcol_d = din("pcol", [128, 1])
    IOA = bass.IndirectOffsetOnAxis
    with ExitStack() as st:
        def V(fn, r, w):
            P.op("dve", fn, reads=r, writes=w)

        def A(fn, r, w):
            P.op("act", fn, reads=r, writes=w)

        def G(fn, r, w):
            P.op("pool", fn, reads=r, writes=w)

        S1i = sb("E_S1i", [128, 64], I32, st)
        S2i = sb("E_S2i", [128, 64], I32, st)
        WIDX = sb("E_widx", [128, 64], I32, st)
        gfbc = sb("E_gfbc", [128, 1024], F32, st)
        P.dma("sp", lambda e: e.dma_start(out=gfbc[:], in_=gfbc_d), "E_gf", writes=["E_gfbc"])
        xn2p_t = xn2p_d.rearrange("(t j p) d -> t p j d", j=4, p=128)
        fl3 = lambda t: t[:].rearrange("p a b -> p (a b)")

        with ExitStack() as sp:
            OHb = sb("E_OHb", [128, 2048], BF16, sp)
            triu = sb("E_triu", [128, 128], BF16, sp)
            onesb = sb("E_onesb", [128, 128], BF16, sp)
            TOT = sb("E_TOT", [128, 64, 32], F32, sp)
            XA = sb("E_XA", [128, 64, 32], F32, sp)
            XB = sb("E_XB", [128, 64, 32], F32, sp)
            SL = sb("E_SL", [128, 64, 32], F32, sp)
            tmp = sb("E_tmp", [128, 64, 32], F32, sp)
            m512 = sb("E_m512", [128, 32, 32], F32, sp)
            t512 = sb("E_t512", [128, 32, 64], F32, sp)
            cmp1 = sb("E_cmp1", [128, 32, 32], F32, sp)
            cmp2 = sb("E_cmp2", [128, 32, 64], F32, sp)
            pcol = sb("E_pcol", [128, 1], F32, sp)
            CE = sb("E_CE", [128, 32], F32, sp)
            Pc = sb("E_Pc", [128, 32], F32, sp)
            BEND = sb("E_BEND", [128, 32], F32, sp)
            BASE = sb("E_BASE", [128, 32], F32, sp)
            ones32 = sb("E_ones32", [128, 32], F32, sp)
            zc = sb("E_zc", [128, 1], F32, sp)
            te = sb("E_te", [128, 64], F32, sp)
            wf = sb("E_wf", [128, 64], F32, sp)
            s1f = sb("E_s1f", [128, 64], F32, sp)
            s2f = sb("E_s2f", [128, 64], F32, sp)
            xq = [sb("E_pxq%d" % i, [128, 4, 1024], BF16, sp) for i in range(2)]
            P.dma("sp", lambda e: e.dma_start(out=triu[:], in_=triu_d), "E_triu", writes=["E_triu"])
            P.dma("sp", lambda e: e.dma_start(out=onesb[:], in_=onesb_d), "E_onesb", writes=["E_onesb"])
            P.dma("sp", lambda e: e.dma_start(out=fl3(m512), in_=m512_d), "E_m512", writes=["E_m512"])
            P.dma("sp", lambda e: e.dma_start(out=fl3(t512), in_=t512_d), "E_t512", writes=["E_t512"])
            P.dma("sp", lambda e: e.dma_start(out=pcol[:], in_=pcol_d), "E_pcol", writes=["E_pcol"])
            V(lambda e: e.memset(ones32[:], 1.0), [], ["E_ones32"])
            V(lambda e: e.memset(zc[:], 0.0), [], ["E_zc"])
            V(lambda e: e.tensor_tensor(out=OHb[:], in0=fl3(M1), in1=fl3(M2), op=ALU.add), ["M1", "M2"], ["E_OHb"])
            for c in range(4):
                P.op("pe", lambda e, c=c: e.matmul(ps[c][:, :], lhsT=triu[:, :], rhs=OHb[:, c * 512:(c + 1) * 512], start=True, stop=True),
                     reads=["E_triu", "E_OHb"], writes=["psR%d" % c])
                P.op("pe", lambda e, c=c: e.matmul(ps[4 + c][:, :], lhsT=onesb[:, :], rhs=OHb[:, c * 512:(c + 1) * 512], start=True, stop=True),
                     reads=["E_onesb", "E_OHb"], writes=["psT%d" % c])
                A(lambda e, c=c: e.copy(out=fl3(TOT)[:, c * 512:(c + 1) * 512], in_=ps[4 + c][:, :]), ["psT%d" % c], ["E_TOT"])
            cur, curn = TOT, ["E_TOT"]
            bufs = [(XA, "E_XA"), (XB, "E_XB")]
            d = 1
            k = 0
            while d < 64:
                nxt, nxtn = bufs[k % 2]
                w = d * 32
                V(lambda e, cur=cur, nxt=nxt, w=w: e.tensor_tensor(out=fl3(nxt)[:, w:2048], in0=fl3(cur)[:, w:2048], in1=fl3(cur)[:, 0:2048 - w], op=ALU.add),
                  curn, [nxtn])
                A(lambda e, cur=cur, nxt=nxt, w=w: e.copy(out=fl3(nxt)[:, 0:w], in_=fl3(cur)[:, 0:w]), curn, [nxtn + "h"])
                cur, curn = nxt, [nxtn, nxtn + "h"]
                d *= 2
                k += 1
            INC, INCn = cur, curn
            EXC, EXCn = XA, "E_XA"
            V(lambda e: e.tensor_tensor(out=fl3(EXC), in0=fl3(INC), in1=fl3(TOT), op=ALU.subtract), INCn + ["E_TOT"], [EXCn, "E_XAh"])
            Cn = INC[:, 63, :]
            V(lambda e: e.tensor_tensor(out=cmp1[:], in0=Cn.unsqueeze(2).to_broadcast([128, 32, 32]), in1=m512[:], op=ALU.is_gt), INCn + ["E_m512"], ["E_cmp1"])
            V(lambda e: e.tensor_reduce(out=CE[:], in_=cmp1[:], op=ALU.add, axis=AX.X), ["E_cmp1"], ["E_CE"])
            V(lambda e: e.tensor_scalar(out=Pc[:], in0=CE[:], scalar1=512.0, scalar2=None, op0=ALU.mult), ["E_CE"], ["E_Pc"])
            V(lambda e: e.tensor_tensor_scan(out=BEND[:], data0=ones32[:], data1=Pc[:], initial=zc[:, 0:1], op0=ALU.mult, op1=ALU.add),
              ["E_Pc", "E_ones32", "E_zc"], ["E_BEND"])
            V(lambda e: e.tensor_tensor(out=BASE[:], in0=BEND[:], in1=Pc[:], op=ALU.subtract), ["E_BEND", "E_Pc"], ["E_BASE"])
            V(lambda e: e.tensor_tensor(out=cmp2[:], in0=BEND[:, :].unsqueeze(2).to_broadcast([128, 32, 64]), in1=t512[:], op=ALU.is_le), ["E_BEND", "E_t512"], ["E_cmp2"])
            V(lambda e: e.tensor_reduce(out=te[:], in_=cmp2[:].rearrange("p e t -> p t e"), op=ALU.add, axis=AX.X), ["E_cmp2"], ["E_te"])
            V(lambda e: e.tensor_scalar(out=wf[:], in0=te[:], scalar1=128.0, scalar2=pcol[:, 0:1], op0=ALU.mult, op1=ALU.add), ["E_te", "E_pcol"], ["E_wf"])
            V(lambda e: e.tensor_copy(out=WIDX[:], in_=wf[:]), ["E_wf"], ["E_widx"])
            for c in range(4):
                V(lambda e, c=c: e.tensor_tensor(out=fl3(SL)[:, c * 512:(c + 1) * 512], in0=ps[c][:, :], in1=fl3(EXC)[:, c * 512:(c + 1) * 512], op=ALU.add),
                  ["psR%d" % c, EXCn], ["E_SL"])
            V(lambda e: e.tensor_tensor(out=SL[:].rearrange("p t e -> p e t"), in0=SL[:].rearrange("p t e -> p e t"),
                                        in1=BASE[:, :].unsqueeze(2).to_broadcast([128, 32, 64]), op=ALU.add), ["E_SL", "E_BASE"], ["E_SL"])
            V(lambda e: e.tensor_tensor(out=fl3(tmp), in0=fl3(M1), in1=fl3(SL), op=ALU.mult), ["M1", "E_SL"], ["E_tmp"])
            V(lambda e: e.tensor_reduce(out=s1f[:], in_=tmp[:], op=ALU.add, axis=AX.X), ["E_tmp"], ["E_s1f"])
            V(lambda e: e.tensor_copy(out=S1i[:], in_=s1f[:]), ["E_s1f"], ["E_S1i"])
            V(lambda e: e.tensor_tensor(out=fl3(tmp), in0=fl3(M2), in1=fl3(SL), op=ALU.mult), ["M2", "E_SL", "E_s1f"], ["E_tmp"])
            V(lambda e: e.tensor_reduce(out=s2f[:], in_=tmp[:], op=ALU.add, axis=AX.X), ["E_tmp"], ["E_s2f"])
            V(lambda e: e.tensor_copy(out=S2i[:], in_=s2f[:]), ["E_s2f"], ["E_S2i"])
            if dbg and "dumpE" in dbg:
                for nm_, t_ in (("dbg_S1i", S1i), ("dbg_S2i", S2i), ("dbg_widx", WIDX)):
                    dd = nc.dram_tensor(nm_, [128, 64], I32, kind="ExternalOutput").ap()
                    P.dma("sp", lambda e, dd=dd, t_=t_: e.dma_start(out=dd, in_=t_[:]), nm_, reads=["E_S1i", "E_S2i", "E_widx"])
            for T in range(NT):
                xb_ = xq[T % 2]
                xbn = "E_pxq%d" % (T % 2)
                P.dma("sp", lambda e, T=T, xb_=xb_: e.dma_start(out=xb_[:], in_=xn2p_t[T]), xbn, writes=[xbn])
                for j in range(4):
                    idx = T * 4 + j
                    for (Si, Sn) in ((S1i, "E_S1i"), (S2i, "E_S2i")):
                        P.dma("pool", lambda e, xb_=xb_, j=j, idx=idx, Si=Si: e.indirect_dma_start(
                            out=Xs_d[:, :], out_offset=IOA(ap=Si[:, idx:idx + 1], axis=0), in_=xb_[:, j, :], in_offset=None),
                            "E_sc%d" % (T % 2), reads=[xbn, Sn])
            P.barrier()
            P.emit()

        with ExitStack() as sx:
            NWB = 3
            w1b = [sb("E_w1b%d" % i, [128, 8, 4, 128], BF16, sx) for i in range(NWB)]
            w3b = [sb("E_w3b%d" % i, [128, 8, 4, 128], BF16, sx) for i in range(NWB)]
            w2b = [sb("E_w2b%d" % i, [128, 4, 1024], BF16, sx) for i in range(NWB)]
            xs_ = [sb("E_xs%d" % i, [128, 4, 1024], BF16, sx) for i in range(2)]
            xT = [sb("E_xT%d" % i, [128, 8, 512], BF16, sx) for i in range(2)]
            hidT = sb("E_hidT", [128, 4, 512], BF16, sx)
            sil = [sb("E_sil%d" % i, [128, 512], F32, sx) for i in range(2)]
            yq = [sb("E_yq%d" % i, [128, 4, 1024], BF16, sx) for i in range(2)]
            Xs_t = Xs_d.rearrange("(t j p) d -> t p j d", j=4, p=128)
            Ys_t = Ys_d.rearrange("(t j p) d -> t p j d", j=4, p=128)

            bcreg = {}

            def wfetch(e, dst, wv, t):
                if "r" not in bcreg:
                    bcreg["r"] = nc.alloc_register(mybir.EngineType.Pool, "wbound")
                    e.reg_mov(bcreg["r"], 4095)
                return e.indirect_dma_start(out=dst, out_offset=None, in_=wv[:, :], in_offset=IOA(ap=WIDX[:, t:t + 1], axis=0),
                                            bounds_check=bcreg["r"], oob_is_err=False)

            def fetch(t):
                wb = t % NWB
                for (dst, wv, nm_) in ((w1b[wb][:].rearrange("p a b c -> p (a b c)"), w1b_d, "E_w1b%d" % wb),
                                       (w3b[wb][:].rearrange("p a b c -> p (a b c)"), w3b_d, "E_w3b%d" % wb),
                                       (w2b[wb][:].rearrange("p a b -> p (a b)"), w2b_d, "E_w2b%d" % wb)):
                    P.dma("pool", lambda e, dst=dst, wv=wv, t=t: wfetch(e, dst, wv, t), nm_ + "f", reads=["E_widx"], writes=[nm_])

            def load_x(t):
                P.dma("sp", lambda e, t=t: e.dma_start(out=xs_[t % 2][:], in_=Xs_t[t]), "E_xs%d" % (t % 2), writes=["E_xs%d" % (t % 2)])

            def transposes(t):
                xb_ = xs_[t % 2]
                for kc in range(8):
                    bi = 6 + kc % 2
                    pv = ps[bi][:].bitcast(BF16)
                    for j in range(4):
                        P.op("pe", lambda e, pv=pv, j=j, kc=kc: e.transpose(out=pv[:, j * 128:(j + 1) * 128], in_=xb_[:, j, kc * 128:(kc + 1) * 128], identity=identb[:]),
                             reads=["E_xs%d" % (t % 2), "identb"], writes=["psX%d" % (kc % 2)])
                    if kc % 2 == 0:
                        A(lambda e, pv=pv, kc=kc: e.copy(out=xT[t % 2][:, kc, :], in_=pv[:, 0:512]), ["psX%d" % (kc % 2)], ["E_xT%d_%d" % (t % 2, kc)])
                    else:
                        V(lambda e, pv=pv, kc=kc: e.tensor_copy(out=xT[t % 2][:, kc, :], in_=pv[:, 0:512]), ["psX%d" % (kc % 2)], ["E_xT%d_%d" % (t % 2, kc)])

            def hidden(t):
                wb = t % NWB
                xres = ["E_xT%d_%d" % (t % 2, kc) for kc in range(8)]
                for fc in range(4):
                    b1 = ps[(2 * fc) % 4]
                    b3 = ps[(2 * fc + 1) % 4]
                    n1, n3 = "psG%d" % ((2 * fc) % 4), "psG%d" % ((2 * fc + 1) % 4)
                    sl = sil[fc % 2]
                    sn_ = "E_sil%d" % (fc % 2)
                    for kc in range(8):
                        P.op("pe", lambda e, b1=b1, kc=kc, fc=fc: e.matmul(b1[:, :], lhsT=w1b[wb][:, kc, fc, :], rhs=xT[t % 2][:, kc, :], start=(kc == 0), stop=(kc == 7)),
                             reads=(["E_w1b%d" % wb] + xres) if kc == 0 else [], writes=[n1])
                    for kc in range(8):
                        P.op("pe", lambda e, b3=b3, kc=kc, fc=fc: e.matmul(b3[:, :], lhsT=w3b[wb][:, kc, fc, :], rhs=xT[t % 2][:, kc, :], start=(kc == 0), stop=(kc == 7)),
                             reads=(["E_w3b%d" % wb] + xres) if kc == 0 else [], writes=[n3])
                    A(lambda e, b1=b1, sl=sl: e.activation(out=sl[:], in_=b1[:, :], func=AF.Silu), [n1], [sn_])
                    V(lambda e, b3=b3, sl=sl, fc=fc: e.tensor_tensor(out=hidT[:, fc, :], in0=b3[:, :], in1=sl[:], op=ALU.mult), [n3, sn_], ["E_hidT%d" % fc])

            def outproj(t):
                wb = t % NWB
                hres = ["E_hidT%d" % fc for fc in range(4)]
                yb_ = yq[t % 2]
                ybn = "E_yq%d" % (t % 2)
                for j in range(4):
                    for hf in range(2):
                        bi = 4 + hf
                        bk = ps[bi]
                        bn = "psH%d" % hf
                        for fc in range(4):
                            P.op("pe", lambda e, bk=bk, fc=fc, j=j, hf=hf: e.matmul(bk[:, :], lhsT=hidT[:, fc, j * 128:(j + 1) * 128], rhs=w2b[wb][:, fc, hf * 512:(hf + 1) * 512],
                                                                                 start=(fc == 0), stop=(fc == 3)),
                                 reads=(hres + ["E_w2b%d" % wb]) if fc == 0 else [], writes=[bn])
                        if hf == 0:
                            A(lambda e, bk=bk, j=j: e.copy(out=yb_[:, j, 0:512], in_=bk[:, :]), [bn], [ybn + "_%d_0" % j])
                        else:
                            V(lambda e, bk=bk, j=j: e.tensor_copy(out=yb_[:, j, 512:1024], in_=bk[:, :]), [bn], [ybn + "_%d_1" % j])
                P.dma("act", lambda e: e.dma_start(out=Ys_t[t], in_=yb_[:]), "E_yso%d" % (t % 2), reads=[ybn + "_%d_%d" % (j, hf) for j in range(4) for hf in range(2)])

            for t in range(NWB):
                fetch(t)
            load_x(0)
            transposes(0)
            for t in range(NTL):
                if t + 1 < NTL:
                    load_x(t + 1)
                hidden(t)
                if t + 1 < NTL:
                    transposes(t + 1)
                outproj(t)
                if t + NWB < NTL:
                    fetch(t + NWB)
            P.barrier()
            P.emit()

        with ExitStack() as sc:
            Y1 = [sb("E_Y1_%d" % i, [128, 1024], BF16, sc) for i in range(2)]
            Y2 = [sb("E_Y2_%d" % i, [128, 1024], BF16, sc) for i in range(2)]
            x1b = [sb("E_x1b%d" % i, [128, 1024], F32, sc) for i in range(2)]
            ob_ = [sb("E_ob%d" % i, [128, 1024], F32, sc) for i in range(2)]
            junk = sb("E_junk", [128, 1024], BF16, sc)
            fs = sb("E_fs", [128, 4], F32, sc)
            x1_r = x1_d.rearrange("(n p) d -> n p d", p=128)
            out_r = out_d.rearrange("(n p) d -> n p d", p=128)
            for n_ in range(64):
                b = n_ % 2
                y1, y2, xb, ob = Y1[b], Y2[b], x1b[b], ob_[b]
                y1n, y2n, xbn, obn = "E_Y1_%d" % b, "E_Y2_%d" % b, "E_x1b%d" % b, "E_ob%d" % b
                P.dma("pool", lambda e, y1=y1, n_=n_: e.indirect_dma_start(out=y1[:, :], out_offset=None, in_=Ys_d[:, :], in_offset=IOA(ap=S1i[:, n_:n_ + 1], axis=0)), y1n, reads=["E_S1i"], writes=[y1n])
                P.dma("pool", lambda e, y2=y2, n_=n_: e.indirect_dma_start(out=y2[:, :], out_offset=None, in_=Ys_d[:, :], in_offset=IOA(ap=S2i[:, n_:n_ + 1], axis=0)), y2n, reads=["E_S2i"], writes=[y2n])
                P.dma("sp", lambda e, xb=xb, n_=n_: e.dma_start(out=xb[:], in_=x1_r[n_]), xbn, writes=[xbn])
                V(lambda e, y1=y1, xb=xb, n_=n_: e.scalar_tensor_tensor(out=xb[:], in0=y1[:], scalar=cw1[:, n_:n_ + 1], in1=xb[:], op0=ALU.mult, op1=ALU.add),
                  [y1n, xbn, "cw1"], [xbn])
                V(lambda e, y2=y2, xb=xb, n_=n_: e.scalar_tensor_tensor(out=xb[:], in0=y2[:], scalar=cw2[:, n_:n_ + 1], in1=xb[:], op0=ALU.mult, op1=ALU.add),
                  [y2n, xbn, "cw2"], [xbn])
                A(lambda e, xb=xb: e.activation(out=junk[:], in_=xb[:], func=AF.Square, accum_out=fs[:, 0:1]), [xbn], ["E_junk", "E_fs"])
                A(lambda e: e.activation(out=fs[:, 1:2], in_=fs[:, 0:1], func=AF.Sqrt, scale=1.0 / D, bias=EPS), ["E_fs"], ["E_fs"])
                V(lambda e: e.reciprocal(out=fs[:, 2:3], in_=fs[:, 1:2]), ["E_fs"], ["E_fs"])
                V(lambda e, xb=xb, ob=ob: e.scalar_tensor_tensor(out=ob[:], in0=xb[:], scalar=fs[:, 2:3], in1=gfbc[:], op0=ALU.mult, op1=ALU.mult),
                  [xbn, "E_fs", "E_gfbc"], [obn])
                P.dma("act", lambda e, ob=ob, n_=n_: e.dma_start(out=out_r[n_], in_=ob[:]), obn + "o", reads=[obn])
            P.barrier()
            P.emit()

    es.close()
    return nc


def _host_consts():
    c = {}
    c["identb"] = np.eye(128, dtype=np.float32).astype(ml_dtypes.bfloat16)
    qt = np.arange(64)[:, None]; jj = np.arange(32)[None, :]
    em = np.where(jj < qt // 2, 0.0, -1e30).astype(np.float32)
    c["emask"] = np.ascontiguousarray(np.broadcast_to(em.reshape(1, 2048), (128, 2048)))
    c["emaskm"] = np.ascontiguousarray(np.broadcast_to((em + np.float32(NEG)).reshape(1, 2048), (128, 2048)))
    o0 = np.where(jj == qt // 2, 0.0, NEG).astype(np.float32)
    c["own0"] = np.ascontiguousarray(np.broadcast_to(o0.reshape(1, 2048), (128, 2048)))
    c["identf"] = np.eye(128, dtype=np.float32)
    c["triu"] = np.triu(np.ones((128, 128), np.float32), 1).astype(ml_dtypes.bfloat16)
    c["onesb"] = np.ones((128, 128), np.float32).astype(ml_dtypes.bfloat16)
    c["m512"] = np.ascontiguousarray(np.broadcast_to((np.arange(32, dtype=np.float32) * 512.0)[None, None, :], (128, 32, 32)).reshape(128, 1024))
    c["t512"] = np.ascontiguousarray(np.broadcast_to((np.arange(64, dtype=np.float32) * 512.0)[None, None, :], (128, 32, 64)).reshape(128, 2048))
    c["pcol"] = np.arange(128, dtype=np.float32).reshape(128, 1)
    c["identr"] = np.eye(128, dtype=np.float32)[::-1].copy().astype(ml_dtypes.bfloat16)
    ind = (np.arange(S)[None, :] // 256 == np.arange(32)[:, None]).astype(np.float32)
    c["ind"] = ind.astype(ml_dtypes.bfloat16)
    n = np.arange(-512, 640)
    nn = np.maximum(n, 0)
    large = 16 + (np.log(np.maximum(nn, 16).astype(np.float32) / np.float32(16)) / np.float32(math.log(128 / 16)) * np.float32(16)).astype(np.int32)
    bucket = np.where(nn < 16, nn, np.minimum(large, 31))
    ohp = np.zeros((33, n.size), np.float32)
    for i, (ni, b) in enumerate(zip(n, bucket)):
        if ni >= 0:
            ohp[b, i] += 1.0
            ohp[31, i] -= 1.0
        else:
            ohp[32, i] = 1.0
    c["ohp"] = ohp
    return c


def _prep(inputs):
    f = lambda a: np.ascontiguousarray(np.asarray(a, dtype=np.float32))
    shared = {}
    shared["w_in"] = f(inputs["w_in"][0])
    shared["g1"] = f(np.asarray(inputs["ln1_g"][0]).reshape(8, 128).T)
    shared["bgate"] = f(np.asarray(inputs["b_gate"][0]).reshape(16, 128).T)
    shared.update(_host_consts())
    shared["w_up_ssm"] = f(inputs["w_up_ssm"][0]); shared["w_up_attn"] = f(inputs["w_up_attn"][0]); shared["w_out"] = f(inputs["w_out"][0])
    shared["g2bc"] = f(np.broadcast_to(np.asarray(inputs["ln2_g"][0])[None, :], (128, 1024)))
    shared["gfbc"] = f(np.broadcast_to(np.asarray(inputs["ln_f_g"])[None, :], (128, 1024)))
    shared["wr"] = f(np.concatenate([inputs["w_router_group"][0], inputs["w_router_expert"][0]], axis=1))
    shared["bias36"] = f(np.broadcast_to(np.concatenate([inputs["b_router_group"][0], inputs["b_router_expert"][0]])[None, :], (128, 36)))
    shared["w1"] = f(inputs["w1"][0]); shared["w3"] = f(inputs["w3"][0]); shared["w2"] = f(inputs["w2"][0])
    rb = f(inputs["rel_bias"])
    shared["rb33"] = f(np.concatenate([rb.T, np.full((1, 8), NEG, np.float32)], axis=0))
    shared["rb31"] = f(np.broadcast_to(rb[:, 31][None, :], (128, 8)))
    lre = f(inputs["ssm_lambda_re"][0]); lim = f(inputs["ssm_lambda_im"][0]); lst = f(inputs["ssm_log_step"][0])
    bre = f(inputs["ssm_b_re"][0]); bim = f(inputs["ssm_b_im"][0])
    cre = f(inputs["ssm_c_re"][0]); cim = f(inputs["ssm_c_im"][0])
    ml = lambda a: f(a.reshape(16, 2, 64).transpose(1, 2, 0).reshape(128, 16))
    shared["lre_ml"] = ml(lre); shared["lim_ml"] = ml(lim)
    shared["lst_ml"] = ml(np.repeat(lst[:, None], 64, axis=1))
    def fl(a):
        t = a.reshape(16, 2, 64).reshape(16, 128)
        return f(np.broadcast_to(t[None], (128, 16, 128)).reshape(128, 2048))
    shared["lre_fl"] = fl(lre); shared["lim_fl"] = fl(lim)
    shared["lst_fl"] = fl(np.repeat(lst[:, None], 64, axis=1))
    def bfl(b):
        o = np.zeros((128, 16, 128), np.float32)
        for k in range(16):
            for two in range(2):
                g = 2 * k + two
                r0 = 32 * (k % 4) + 16 * two
                o[r0:r0 + 16, k, two * 64:(two + 1) * 64] = b[g].T
        return o.reshape(128, 2048)
    shared["bre_fl"] = bfl(bre); shared["bim_fl"] = bfl(bim)
    def cml(c):
        o = np.zeros((128, 16, 128), np.float32)
        for k in range(16):
            for two in range(2):
                g = 2 * k + two
                c0 = 32 * (k % 4) + 16 * two
                o[two * 64:(two + 1) * 64, k, c0:c0 + 16] = c[g].T
        return o.reshape(128, 2048)
    shared["cre_ml"] = cml(cre); shared["cim_ml"] = cml(cim)
    shared["d_fm"] = f(np.asarray(inputs["ssm_d"][0]).reshape(4, 128).T)
    shared["wglu"] = f(inputs["w_glu"][0])
    shared["bglu"] = f(np.asarray(inputs["b_glu"][0]).reshape(4, 128).T)
    x = np.asarray(inputs["x"], dtype=np.float32)
    maps = []
    for c in range(8):
        m = dict(shared)
        m["x"] = np.ascontiguousarray(x[c])
        maps.append(m)
    return maps


def kernel(**inputs):
    nc = build()
    maps = _prep(inputs)
    res = run_bass_kernel_spmd(nc, maps, core_ids=list(range(8)))
    out = np.stack([np.asarray(r["out"], dtype=np.float32) for r in res.results], axis=0)
    return out
```
